# Optimizing a Trainium2 kernel written in Bass

```python
import math
import jax, jax.numpy as jnp
from jax import lax
import numpy as np

D_MODEL = 2048
BATCH = 2
SEQ = 8192
DEPTH = 1
DEC_BATCH = 16
DEC_SEQ = 16
PAST_LEN = 4096

CHUNK = 64
A_HEADS = 8
A_KV_HEADS = 2
A_REP = A_HEADS // A_KV_HEADS
A_HEAD_DIM = 128
IDX_HEADS = 16
IDX_DIM = 64
TOPK_MAX = 256
B_HEADS = 4
B_HEAD_DIM = 128
N_REL_BUCKETS = 32
REL_MAX_DIST = 128
N_ATTN_HEADS = A_HEADS + B_HEADS
IN_SIZES = (A_HEADS * A_HEAD_DIM, A_KV_HEADS * A_HEAD_DIM, A_KV_HEADS * A_HEAD_DIM,
            IDX_HEADS * IDX_DIM, IDX_DIM, IDX_HEADS,
            B_HEADS * 2 * B_HEAD_DIM, B_HEADS * 2 * B_HEAD_DIM, B_HEADS * 2 * B_HEAD_DIM)
D_IN = sum(IN_SIZES)
MIX_WIDTH = A_HEADS * A_HEAD_DIM + B_HEADS * 2 * B_HEAD_DIM
N_EXPERTS = 64
TOP_K = 8
N_GROUPS = 8
TOPK_GROUPS = 4
EXPERT_DIM = 512
SHARED_DIM = 512
ROUTED_SCALE = 2.5
EPS = 1e-6
QBLOCK = 128

kernel_name = "hybrid_dsa_diffattn_moe_stream_step"


def _rmsnorm(x, g):
    xf = x.astype(jnp.float32)
    y = xf * lax.rsqrt(jnp.mean(xf * xf, axis=-1, keepdims=True) + EPS)
    return (y * g.astype(jnp.float32)).astype(x.dtype)


def _rel_bucket(rel):
    nb = N_REL_BUCKETS // 2
    max_exact = nb // 2
    n = jnp.abs(rel)
    nf = jnp.maximum(n, 1).astype(jnp.float32)
    large = max_exact + (jnp.log(nf / max_exact) / math.log(REL_MAX_DIST / max_exact)
                         * (nb - max_exact)).astype(jnp.int32)
    large = jnp.minimum(large, nb - 1)
    return jnp.where(rel > 0, nb, 0) + jnp.where(n < max_exact, n, large)


def _split_cols(proj):
    offs = [int(o) for o in np.cumsum(IN_SIZES)[:-1]]
    return jnp.split(proj, offs, axis=-1)


def _sweep(fn, q_arrays, qpos):
    B, T = q_arrays[0].shape[:2]
    qb = min(QBLOCK, T)
    nb = T // qb
    blocks = tuple(jnp.swapaxes(a.reshape((B, nb, qb) + a.shape[2:]), 0, 1) for a in q_arrays)
    out = lax.map(lambda xs: fn(*xs), blocks + (qpos.reshape(nb, qb),))
    return jnp.swapaxes(out, 0, 1).reshape(B, T, out.shape[-1])


def _dsa_block(qa, qi, iw, qpos, ka, va, ki, kpos, bias_tab, topk):
    B, QB = qa.shape[:2]
    visible = (kpos[None, :] // CHUNK) <= (qpos[:, None] // CHUNK)
    dots = jnp.einsum('bqhd,bsd->bqhs', qi, ki).astype(jnp.float32) * IDX_DIM ** -0.5
    score = jnp.einsum('bqh,bqhs->bqs', iw.astype(jnp.float32) * IDX_HEADS ** -0.5, jax.nn.relu(dots))
    score = jnp.where(visible[None], score, -jnp.inf)
    _, sel = lax.top_k(score, topk)
    bidx = jnp.arange(B)[:, None, None]
    ks = ka[bidx, sel]
    vs = va[bidx, sel]
    sel_pos = kpos[sel]
    valid = (sel_pos // CHUNK) <= (qpos[None, :, None] // CHUNK)
    bias = bias_tab[_rel_bucket(sel_pos - qpos[None, :, None])][..., :A_HEADS]
    bias = bias.reshape(B, QB, topk, A_KV_HEADS, A_REP).transpose(0, 1, 3, 4, 2)
    q = qa.reshape(B, QB, A_KV_HEADS, A_REP, A_HEAD_DIM)
    logits = jnp.einsum('bqgrd,bqkgd->bqgrk', q, ks).astype(jnp.float32) * A_HEAD_DIM ** -0.5 + bias
    logits = jnp.where(valid[:, :, None, None, :], logits, -jnp.inf)
    p = jax.nn.softmax(logits, axis=-1).astype(vs.dtype)
    out = jnp.einsum('bqgrk,bqkgd->bqgrd', p, vs)
    return out.reshape(B, QB, A_HEADS * A_HEAD_DIM)


def _diff_block(qb, qpos, kb, vb, kpos, bias_tab, lam, lam_init, subln_g):
    B, QB = qb.shape[:2]
    visible = (kpos[None, :] // CHUNK) <= (qpos[:, None] // CHUNK)
    logits = jnp.einsum('bqhcd,bshcd->bhcqs', qb, kb).astype(jnp.float32) * B_HEAD_DIM ** -0.5
    bias = bias_tab[_rel_bucket(kpos[None, :] - qpos[:, None])][..., A_HEADS:]
    logits = logits + jnp.transpose(bias, (2, 0, 1))[None, :, None]
    logits = jnp.where(visible, logits, -jnp.inf)
    p = jax.nn.softmax(logits, axis=-1)
    a = (p[:, :, 0] - lam * p[:, :, 1]).astype(vb.dtype)
    o = jnp.einsum('bhqs,bshe->bqhe', a, vb)
    o = _rmsnorm(o, subln_g) * (1.0 - lam_init)
    return o.reshape(B, QB, B_HEADS * 2 * B_HEAD_DIM)


def _swiglu(t, wg, wu, wd):
    return (jax.nn.silu(t @ wg) * (t @ wu)) @ wd


def _moe(h, w_router, router_bias, w_gate, w_up, w_down, ws_gate, ws_up, ws_down):
    B, T, D = h.shape
    t = h.reshape(B * T, D)
    scores = jax.nn.sigmoid((t @ w_router).astype(jnp.float32))
    choice = scores + router_bias.astype(jnp.float32)
    grp = choice.reshape(-1, N_GROUPS, N_EXPERTS // N_GROUPS)
    grp_score = lax.top_k(grp, 2)[0].sum(-1)
    _, gidx = lax.top_k(grp_score, TOPK_GROUPS)
    gmask = jax.nn.one_hot(gidx, N_GROUPS, dtype=jnp.float32).sum(-2)
    emask = jnp.repeat(gmask, N_EXPERTS // N_GROUPS, axis=-1)
    _, eidx = lax.top_k(jnp.where(emask > 0, choice, -jnp.inf), TOP_K)
    sel = jnp.take_along_axis(scores, eidx, axis=-1)
    wts = sel / jnp.sum(sel, axis=-1, keepdims=True) * ROUTED_SCALE
    gates = jnp.sum(jax.nn.one_hot(eidx, N_EXPERTS, dtype=jnp.float32) * wts[..., None], axis=-2)

    def body(acc, xs):
        wg, wu, wd, g = xs
        return acc + g[:, None].astype(t.dtype) * _swiglu(t, wg, wu, wd), None

    acc, _ = lax.scan(body, _swiglu(t, ws_gate, ws_up, ws_down), (w_gate, w_up, w_down, gates.T))
    return acc.reshape(B, T, D)


def _layer(x, c, past, layer_idx, rel_bias, w_ada, b_ada, norm_a_g, w_in, w_out, diff_lam, subln_g,
           norm_f_g, w_router, router_bias, w_gate, w_up, w_down, ws_gate, ws_up, ws_down):
    B, T, _ = x.shape
    mod = (jax.nn.silu(c) @ w_ada + b_ada)[:, None, :]
    sh_a, sc_a, g_a, sh_f, sc_f, g_f = jnp.split(mod, 6, axis=-1)
    h = _rmsnorm(x, norm_a_g) * (1.0 + sc_a) + sh_a
    qa, ka, va, qi, ki, iw, qb, kb, vb = _split_cols(h @ w_in)
    qa = qa.reshape(B, T, A_HEADS, A_HEAD_DIM)
    ka = ka.reshape(B, T, A_KV_HEADS, A_HEAD_DIM)
    va = va.reshape(B, T, A_KV_HEADS, A_HEAD_DIM)
    qi = qi.reshape(B, T, IDX_HEADS, IDX_DIM)
    qb = qb.reshape(B, T, B_HEADS, 2, B_HEAD_DIM)
    kb = kb.reshape(B, T, B_HEADS, 2, B_HEAD_DIM)
    vb = vb.reshape(B, T, B_HEADS, 2 * B_HEAD_DIM)
    new_rows = (ka, va, ki, kb, vb)
    if past is None:
        offset = 0
        ka_all, va_all, ki_all, kb_all, vb_all = new_rows
    else:
        offset = past[0].shape[1]
        ka_all, va_all, ki_all, kb_all, vb_all = (jnp.concatenate([pc, nr], axis=1) for pc, nr in zip(past, new_rows))
    L = offset + T
    qpos = offset + jnp.arange(T, dtype=jnp.int32)
    kpos = jnp.arange(L, dtype=jnp.int32)
    topk = min(TOPK_MAX, L // 4)

    out_a = _sweep(lambda q_, qi_, iw_, p_: _dsa_block(q_, qi_, iw_, p_, ka_all, va_all, ki_all, kpos, rel_bias, topk),
                   (qa, qi, iw), qpos)
    lam_init = 0.8 - 0.6 * math.exp(-0.3 * layer_idx)
    dl = diff_lam.astype(jnp.float32)
    lam = jnp.exp(jnp.sum(dl[0] * dl[1])) - jnp.exp(jnp.sum(dl[2] * dl[3])) + lam_init
    out_b = _sweep(lambda q_, p_: _diff_block(q_, p_, kb_all, vb_all, kpos, rel_bias, lam, lam_init, subln_g),
                   (qb,), qpos)
    x = x + g_a * (jnp.concatenate([out_a, out_b], axis=-1) @ w_out)
    h = _rmsnorm(x, norm_f_g) * (1.0 + sc_f) + sh_f
    x = x + g_f * _moe(h, w_router, router_bias, w_gate, w_up, w_down, ws_gate, ws_up, ws_down)
    return x, new_rows


def _stack(rows, i):
    return jnp.stack([r[i] for r in rows], axis=0)


def setup_inputs(seed: int = 0) -> dict:
    key = jax.random.key(seed)
    keys = list(jax.random.split(key, 32))

    def nrm(i, shape, s):
        return jax.random.normal(keys[i], shape, jnp.float32) * s

    D = D_MODEL
    return {
        "x_prompt": nrm(0, (BATCH, SEQ, D), 1.0),
        "x_sample": nrm(1, (DEC_BATCH, DEC_SEQ, D), 1.0),
        "c_prompt": nrm(2, (BATCH, D), 1.0),
        "c_sample": nrm(3, (DEC_BATCH, D), 1.0),
        "cache_a_k": nrm(4, (DEPTH, DEC_BATCH, PAST_LEN, A_KV_HEADS, A_HEAD_DIM), 1.0),
        "cache_a_v": nrm(5, (DEPTH, DEC_BATCH, PAST_LEN, A_KV_HEADS, A_HEAD_DIM), 1.0),
        "cache_a_kidx": nrm(6, (DEPTH, DEC_BATCH, PAST_LEN, IDX_DIM), 1.0),
        "cache_b_k": nrm(7, (DEPTH, DEC_BATCH, PAST_LEN, B_HEADS, 2, B_HEAD_DIM), 1.0),
        "cache_b_v": nrm(8, (DEPTH, DEC_BATCH, PAST_LEN, B_HEADS, 2 * B_HEAD_DIM), 1.0),
        "rel_bias": nrm(9, (N_REL_BUCKETS, N_ATTN_HEADS), 0.5),
        "w_ada": nrm(10, (DEPTH, D, 6 * D), 0.5 * D ** -0.5),
        "b_ada": nrm(11, (DEPTH, 6 * D), 0.02),
        "norm_a_g": 1.0 + nrm(12, (DEPTH, D), 0.02),
        "w_in": nrm(13, (DEPTH, D, D_IN), D ** -0.5),
        "w_out": nrm(14, (DEPTH, MIX_WIDTH, D), MIX_WIDTH ** -0.5),
        "diff_lam": nrm(15, (DEPTH, 4, B_HEAD_DIM), 0.1),
        "subln_g": 1.0 + nrm(16, (DEPTH, 2 * B_HEAD_DIM), 0.02),
        "norm_f_g": 1.0 + nrm(17, (DEPTH, D), 0.02),
        "w_router": nrm(18, (DEPTH, D, N_EXPERTS), D ** -0.5),
        "router_bias": nrm(19, (DEPTH, N_EXPERTS), 0.01),
        "w_gate": nrm(20, (DEPTH, N_EXPERTS, D, EXPERT_DIM), D ** -0.5),
        "w_up": nrm(21, (DEPTH, N_EXPERTS, D, EXPERT_DIM), D ** -0.5),
        "w_down": nrm(22, (DEPTH, N_EXPERTS, EXPERT_DIM, D), EXPERT_DIM ** -0.5),
        "ws_gate": nrm(23, (DEPTH, D, SHARED_DIM), D ** -0.5),
        "ws_up": nrm(24, (DEPTH, D, SHARED_DIM), D ** -0.5),
        "ws_down": nrm(25, (DEPTH, SHARED_DIM, D), SHARED_DIM ** -0.5),
        "final_g": 1.0 + nrm(26, (D,), 0.02),
    }


def reference(x_prompt, x_sample, c_prompt, c_sample, cache_a_k, cache_a_v, cache_a_kidx, cache_b_k, cache_b_v,
              rel_bias, w_ada, b_ada, norm_a_g, w_in, w_out, diff_lam, subln_g, norm_f_g, w_router, router_bias,
              w_gate, w_up, w_down, ws_gate, ws_up, ws_down, final_g):
    xp, xs = x_prompt, x_sample
    rows_p, rows_s = [], []
    for l in range(DEPTH):
        lw = (w_ada[l], b_ada[l], norm_a_g[l], w_in[l], w_out[l], diff_lam[l], subln_g[l], norm_f_g[l],
              w_router[l], router_bias[l], w_gate[l], w_up[l], w_down[l], ws_gate[l], ws_up[l], ws_down[l])
        xp, rp = _layer(xp, c_prompt, None, l, rel_bias, *lw)
        past = (cache_a_k[l], cache_a_v[l], cache_a_kidx[l], cache_b_k[l], cache_b_v[l])
        xs, rs = _layer(xs, c_sample, past, l, rel_bias, *lw)
        rows_p.append(rp)
        rows_s.append(rs)
    y_prompt = _rmsnorm(xp, final_g)
    y_sample = _rmsnorm(xs, final_g)
    return (y_prompt, y_sample,
            _stack(rows_p, 0), _stack(rows_p, 1), _stack(rows_p, 2), _stack(rows_p, 3), _stack(rows_p, 4),
            _stack(rows_s, 0), _stack(rows_s, 1), _stack(rows_s, 2), _stack(rows_s, 3), _stack(rows_s, 4))
```

```python
import os
import contextlib
import numpy as np
import concourse.bass as bass
import concourse.mybir as mybir
from concourse.bass_utils import run_bass_kernel_spmd

F32 = mybir.dt.float32
BF16 = mybir.dt.bfloat16
ALU = mybir.AluOpType
AF = mybir.ActivationFunctionType
AX = mybir.AxisListType

D = 2048
DIN = 5712
SEQ = 8192
PAST = 4096
LS = PAST + 16
LSP = 4224
NEXP = 64
EPS = 1e-6
NEGV = -30000.0
STAGE = int(os.environ.get("MK_STAGE", "9"))
NBIS = 20

ENGS = ("pe", "act", "dve", "pool", "sp")


class Op:
    __slots__ = ("eng", "fn", "deps", "is_dma", "sig", "has_dep", "dsem", "dval")

    def __init__(self, eng, fn, is_dma):
        self.eng = eng
        self.fn = fn
        self.is_dma = is_dma
        self.deps = []
        self.sig = None
        self.has_dep = False


class Sched:
    def __init__(self, nc, es, ndma=8):
        self.nc = nc
        self.ndma = ndma
        self.csem = {e: es.enter_context(nc.semaphore("c_" + e)) for e in ENGS}
        self.dsem = {e: [es.enter_context(nc.semaphore("d_%s%d" % (e, i))) for i in range(ndma)]
                     for e in ("sp", "pool")}
        self.cnt = {e: 0 for e in ENGS}
        self.dcnt = {e: 0 for e in self.dsem}
        self.first = True
        self.reset()

    def reset(self):
        self.ops = {e: [] for e in ENGS}
        self.state = {}

    def add(self, eng, fn, reads=(), writes=(), dma=False):
        op = Op(eng, fn, dma)
        deps = []
        for k in reads:
            st = self.state.get(k)
            if st is None:
                st = self.state[k] = [None, []]
            if st[0] is not None:
                deps.append(st[0])
            st[1].append(op)
        for k in writes:
            st = self.state.get(k)
            if st is None:
                st = self.state[k] = [None, []]
            if st[0] is not None:
                deps.append(st[0])
            for r in st[1]:
                if r is not op:
                    deps.append(r)
            st[0] = op
            st[1] = []
        seen = set()
        for d in deps:
            if id(d) in seen or d is op:
                continue
            seen.add(id(d))
            op.deps.append(d)
            d.has_dep = True
        self.ops[eng].append(op)
        return op

    def emit(self):
        nc = self.nc
        start_c = dict(self.cnt)
        start_d = {}
        for e in self.dsem:
            for i in range(self.ndma):
                n_i = (self.dcnt[e] - i + self.ndma - 1) // self.ndma if self.dcnt[e] > i else 0
                start_d[(e, i)] = 16 * n_i
        first = self.first
        self.first = False
        for e in ENGS:
            last = None
            for op in self.ops[e]:
                if not op.is_dma and op.fn is not None:
                    last = op
            if last is not None:
                last.has_dep = True
            for op in self.ops[e]:
                if op.is_dma:
                    n = self.dcnt[e]
                    op.dsem = self.dsem[e][n % self.ndma]
                    op.dval = 16 * (n // self.ndma + 1)
                    self.dcnt[e] = n + 1
                elif op.has_dep:
                    self.cnt[e] += 1
                    op.sig = self.cnt[e]
        ops = self.ops
        csem, dsem, ndma = self.csem, self.dsem, self.ndma

        def run(ename, engobj):
            waited = {}

            def wait(sem, val):
                key = id(sem)
                if val <= 0 or waited.get(key, 0) >= val:
                    return
                waited[key] = val
                engobj.wait_ge(sem, val)

            if not ops[ename]:
                return
            if not first:
                for f in ENGS:
                    wait(csem[f], start_c[f])
                for (q, i), v in start_d.items():
                    wait(dsem[q][i], v)
            for op in ops[ename]:
                for d in op.deps:
                    if d.is_dma:
                        wait(d.dsem, d.dval)
                    else:
                        wait(csem[d.eng], d.sig)
                if op.is_dma:
                    wait(op.dsem, op.dval - 16)
                    ins = op.fn(engobj)
                    ins.then_inc(op.dsem, 16)
                else:
                    ins = op.fn(engobj)
                    if op.sig is not None:
                        ins.then_inc(csem[ename], 1)
            if ename in dsem:
                lastd = {}
                for op in ops[ename]:
                    if op.is_dma:
                        lastd[id(op.dsem)] = (op.dsem, op.dval)
                for s, v in lastd.values():
                    wait(s, v)

        with nc.allow_non_contiguous_dma(reason="small strided parameter loads"):
            with nc.Block() as block:
                @block.tensor
                def _(e):
                    run("pe", e)

                @block.scalar
                def _(e):
                    run("act", e)

                @block.vector
                def _(e):
                    run("dve", e)

                @block.gpsimd
                def _(e):
                    run("pool", e)

                @block.sync
                def _(e):
                    run("sp", e)
        self.reset()


class Rot:
    def __init__(self, name, n):
        self.name, self.n, self.i = name, n, 0

    def next(self):
        i = self.i % self.n
        self.i += 1
        return i, (self.name, i)


def build_program():
    nc = bass.Bass("TRN2", target_bir_lowering=False)

    def din(name, shape, dt=F32):
        return nc.dram_tensor(name, list(shape), dt, kind="ExternalInput").ap()

    def dout(name, shape):
        return nc.dram_tensor(name, list(shape), F32, kind="ExternalOutput").ap()

    def dscr(name, shape, dt):
        return nc.dram_tensor(name, list(shape), dt, kind="Internal").ap()

    xb = din("xb", [SEQ, D])
    xown = din("xown", [2048, D])
    xsmp = din("xsmp", [32, D])
    c3 = din("c3", [3, D])
    cak = din("cak", [2, PAST, 256])
    cav = din("cav", [2, PAST, 256])
    cki = din("cki", [2, PAST, 64])
    cbk = din("cbk", [2, PAST, 1024])
    cbv = din("cbv", [2, PAST, 1024])
    relb = din("relb", [1, 384])
    w_ada = din("w_ada", [D, 6 * D])
    b_ada = din("b_ada", [1, 6 * D])
    norm_a_g = din("norm_a_g", [1, D])
    w_in = din("w_in", [D, DIN])
    w_out = din("w_out", [D, D])
    diff_lam = din("diff_lam", [1, 512])
    subln_g = din("subln_g", [1, 256])
    norm_f_g = din("norm_f_g", [1, D])
    w_router = din("w_router", [D, NEXP])
    router_bias = din("router_bias", [1, NEXP])
    w_gate = din("w_gate", [NEXP, D, 512])
    w_up = din("w_up", [NEXP, D, 512])
    w_down = din("w_down", [NEXP, 512, D])
    ws_gate = din("ws_gate", [D, 512])
    ws_up = din("ws_up", [D, 512])
    ws_down = din("ws_down", [512, D])
    final_g = din("final_g", [1, D])
    cbc = din("cbc", [7, 32, 128 * 128])
    negc = din("negc", [7, 128, 128])
    vis01 = din("vis01", [128, 512])
    visneg = din("visneg", [128, 512])

    y_own = dout("y_own", [2048, D])
    y_smp = dout("y_smp", [32, D])
    ak_own = dout("ak_own", [2048, 256])
    av_own = dout("av_own", [2048, 256])
    ki_own = dout("ki_own", [2048, 64])
    bk_own = dout("bk_own", [2048, 1024])
    bv_own = dout("bv_own", [2048, 1024])
    ak_s = dout("ak_s", [32, 256])
    av_s = dout("av_s", [32, 256])
    ki_s = dout("ki_s", [32, 64])
    bk_s = dout("bk_s", [32, 1024])
    bv_s = dout("bv_s", [32, 1024])

    modS = dscr("modS", [3, 6 * D], F32)
    Ls = [SEQ, LSP, LSP]
    KAT = [dscr("KAT%d" % i, [2, 128, Ls[i]], BF16) for i in range(3)]
    VA = [dscr("VA%d" % i, [Ls[i], 256], BF16) for i in range(3)]
    KIT = [dscr("KIT%d" % i, [64, Ls[i]], BF16) for i in range(3)]
    KBT = [dscr("KBT%d" % i, [8, 128, Ls[i]], BF16) for i in range(3)]
    VB = [dscr("VB%d" % i, [Ls[i], 1024], BF16) for i in range(3)]
    QAT = dscr("QAT", [8, 128, 2080], BF16)
    QBT = dscr("QBT", [8, 128, 2080], BF16)
    QIT = dscr("QIT", [8, 128, 2080], BF16)
    IWS = dscr("IWS", [2080, 16], F32)
    if os.environ.get("MK_DEBUG"):
        mixS = nc.dram_tensor("mixS", [2080, D], BF16, kind="ExternalOutput").ap()
    else:
        mixS = dscr("mixS", [2080, D], BF16)
    if os.environ.get("MK_DEBUG"):
        x1S = nc.dram_tensor("x1S", [2080, D], F32, kind="ExternalOutput").ap()
    else:
        x1S = dscr("x1S", [2080, D], F32)

    with contextlib.ExitStack() as ges:
        S = Sched(nc, ges)

        def gsb(name, shape, dt):
            return ges.enter_context(nc.sbuf_tensor(name, list(shape), dt))

        ident_b = gsb("ident_b", [128, 128], BF16)
        ident_f = gsb("ident_f", [128, 128], F32)
        ones_b = gsb("ones_b", [128, 1], BF16)
        onesrow = gsb("onesrow", [1, 128], F32)
        AaT = gsb("AaT", [128, 3, 16], F32)
        BaT = gsb("BaT", [128, 3, 16], F32)
        AfT = gsb("AfT", [128, 3, 16], F32)
        BfT = gsb("BfT", [128, 3, 16], F32)
        MB = gsb("MB", [128, 7, 12, 128], BF16)
        lamt = gsb("lamt", [128, 4], F32)
        sg8 = gsb("sg8", [128, 256], F32)
        rbias = gsb("rbias", [128, NEXP], F32)

        with contextlib.ExitStack() as es:
            def sb(name, shape, dt):
                return es.enter_context(nc.sbuf_tensor(name, list(shape), dt))

            def ps(name, shape, dt=F32):
                return es.enter_context(nc.psum_tensor(name, list(shape), dt))

            scT = sb("scT", [128, 16, 3], F32)
            wblk = [sb("wblk%d" % i, [128, 16, 512], F32) for i in range(2)]
            bblk = [sb("bblk%d" % i, [1, 512], F32) for i in range(2)]
            mblk = [sb("mblk%d" % i, [3, 512], F32) for i in range(2)]
            pm = [ps("pm%d" % i, [128, 512]) for i in range(2)]
            pmb = ps("pmb", [128, 128, 16])
            cbt = sb("cbt", [32, 128 * 128], F32)
            negt = sb("negt", [128, 7, 128], F32)
            tab = sb("tab", [32, 12], F32)
            t16 = [sb("t16_%d" % i, [128, 3, 16], F32) for i in range(4)]
            g16 = [sb("g16_%d" % i, [128, 16], F32) for i in range(2)]
            dl = sb("dl", [128, 512], F32)
            dtmp = sb("dtmp", [128, 256], F32)
            sgl = sb("sgl", [128, 256], F32)

            A = S.add
            A("pool", lambda e: e.memset(ident_b[:], 1.0), writes=["ident_b"])
            A("pool", lambda e: e.affine_select(out=ident_b[:], in_=ident_b[:], pattern=[[-1, 128]],
                                                compare_op=ALU.is_equal, fill=0.0, base=0, channel_multiplier=1),
              reads=["ident_b"], writes=["ident_b"])
            A("pool", lambda e: e.memset(ident_f[:], 1.0), writes=["ident_f"])
            A("pool", lambda e: e.affine_select(out=ident_f[:], in_=ident_f[:], pattern=[[-1, 128]],
                                                compare_op=ALU.is_equal, fill=0.0, base=0, channel_multiplier=1),
              reads=["ident_f"], writes=["ident_f"])
            A("pool", lambda e: e.memset(ones_b[:], 1.0), writes=["ones_b"])
            A("pool", lambda e: e.memset(onesrow[:], 1.0), writes=["onesrow"])
            for r in range(3):
                A("sp", lambda e, r=r: e.dma_start(out=scT[:, :, r], in_=c3[r].rearrange("(kt p) -> p kt", p=128)),
                  writes=["scT"], dma=True)
            A("act", lambda e: e.activation(out=scT[:], in_=scT[:], func=AF.Silu), reads=["scT"], writes=["scT"])
            wv = w_ada.rearrange("(kt p) n -> p kt n", p=128)
            for nb in range(24):
                i = nb % 2
                A("sp", lambda e, i=i, nb=nb: e.dma_start(out=wblk[i][:], in_=wv[:, :, nb * 512:(nb + 1) * 512]),
                  writes=[("wblk", i)], dma=True)
                A("sp", lambda e, i=i, nb=nb: e.dma_start(out=bblk[i][:], in_=b_ada[:, nb * 512:(nb + 1) * 512]),
                  writes=[("bblk", i)], dma=True)
                for kt in range(16):
                    A("pe", lambda e, i=i, kt=kt: e.matmul(pm[i][0:3, :], lhsT=scT[:, kt, :], rhs=wblk[i][:, kt, :],
                                                           start=(kt == 0), stop=False),
                      reads=["scT", ("wblk", i)], writes=[("pm", i)])
                A("pe", lambda e, i=i: e.matmul(pm[i][0:3, :], lhsT=onesrow[0:1, 0:3], rhs=bblk[i][:],
                                                start=False, stop=True),
                  reads=["onesrow", ("bblk", i)], writes=[("pm", i)])
                A("dve", lambda e, i=i: e.tensor_copy(mblk[i][:], pm[i][0:3, :]), reads=[("pm", i)], writes=[("mblk", i)])
                A("sp", lambda e, i=i, nb=nb: e.dma_start(out=modS[:, nb * 512:(nb + 1) * 512], in_=mblk[i][:]),
                  reads=[("mblk", i)], writes=["modS"], dma=True)
            for ti, off in enumerate((0, 2048, 6144, 8192)):
                for r in range(3):
                    A("sp", lambda e, ti=ti, off=off, r=r: e.dma_start(
                        out=t16[ti][:, r, :], in_=modS[r, off:off + 2048].rearrange("(kt p) -> p kt", p=128)),
                      reads=["modS"], writes=[("t16", ti)], dma=True)
            A("sp", lambda e: e.dma_start(out=g16[0][:], in_=norm_a_g.rearrange("o (kt p) -> p (o kt)", p=128)),
              writes=[("g16", 0)], dma=True)
            A("sp", lambda e: e.dma_start(out=g16[1][:], in_=norm_f_g.rearrange("o (kt p) -> p (o kt)", p=128)),
              writes=[("g16", 1)], dma=True)
            for (dst, dname, sci, shi, gi) in ((AaT, "AaT", 1, 0, 0), (AfT, "AfT", 3, 2, 1)):
                A("dve", lambda e, sci=sci: e.tensor_scalar(out=t16[sci][:], in0=t16[sci][:], scalar1=1.0, scalar2=None,
                                                            op0=ALU.add),
                  reads=[("t16", sci)], writes=[("t16", sci)])
                A("dve", lambda e, dst=dst, sci=sci, gi=gi: e.tensor_tensor(
                    out=dst[:], in0=t16[sci][:], in1=g16[gi][:].unsqueeze(1).to_broadcast([128, 3, 16]), op=ALU.mult),
                  reads=[("t16", sci), ("g16", gi)], writes=[dname])
            A("dve", lambda e: e.tensor_copy(BaT[:], t16[0][:]), reads=[("t16", 0)], writes=["BaT"])
            A("dve", lambda e: e.tensor_copy(BfT[:], t16[2][:]), reads=[("t16", 2)], writes=["BfT"])
            A("sp", lambda e: e.dma_start(out=dl[:], in_=diff_lam.partition_broadcast(128)), writes=["dl"], dma=True)
            A("dve", lambda e: e.tensor_tensor(out=dtmp[:, 0:128], in0=dl[:, 0:128], in1=dl[:, 128:256], op=ALU.mult),
              reads=["dl"], writes=["dtmp0"])
            A("dve", lambda e: e.tensor_tensor(out=dtmp[:, 128:256], in0=dl[:, 256:384], in1=dl[:, 384:512], op=ALU.mult),
              reads=["dl"], writes=["dtmp1"])
            A("dve", lambda e: e.reduce_sum(out=lamt[:, 2:3], in_=dtmp[:, 0:128], axis=AX.X), reads=["dtmp0"], writes=["lam2"])
            A("dve", lambda e: e.reduce_sum(out=lamt[:, 3:4], in_=dtmp[:, 128:256], axis=AX.X), reads=["dtmp1"], writes=["lam3"])
            A("act", lambda e: e.activation(out=lamt[:, 2:4], in_=lamt[:, 2:4], func=AF.Exp), reads=["lam2", "lam3"],
              writes=["lam23"])
            A("dve", lambda e: e.tensor_tensor(out=lamt[:, 0:1], in0=lamt[:, 2:3], in1=lamt[:, 3:4], op=ALU.subtract),
              reads=["lam23"], writes=["lam0"])
            A("dve", lambda e: e.tensor_scalar(out=lamt[:, 0:1], in0=lamt[:, 0:1], scalar1=0.2, scalar2=None, op0=ALU.add),
              reads=["lam0"], writes=["lam0"])
            A("dve", lambda e: e.tensor_scalar(out=lamt[:, 1:2], in0=lamt[:, 0:1], scalar1=-1.0, scalar2=None, op0=ALU.mult),
              reads=["lam0"], writes=["lam1"])
            A("sp", lambda e: e.dma_start(out=sgl[:], in_=subln_g.partition_broadcast(128)), writes=["sgl"], dma=True)
            A("dve", lambda e: e.tensor_scalar(out=sg8[:], in0=sgl[:], scalar1=0.8, scalar2=None, op0=ALU.mult),
              reads=["sgl"], writes=["sg8"])
            A("sp", lambda e: e.dma_start(out=rbias[:], in_=router_bias.partition_broadcast(128)), writes=["rbias"], dma=True)
            A("sp", lambda e: e.dma_start(out=tab[:], in_=relb.rearrange("o (b h) -> (o b) h", h=12)), writes=["tab"], dma=True)
            A("sp", lambda e: e.dma_start(out=negt[:], in_=negc.rearrange("u s q -> s u q")), writes=["negt"], dma=True)
            for u in range(7):
                A("sp", lambda e, u=u: e.dma_start(out=cbt[:], in_=cbc[u]), writes=["cbt"], dma=True)
                for q in range(128):
                    A("pe", lambda e, q=q: e.matmul(pmb[:, q, 0:12], lhsT=cbt[:, q * 128:(q + 1) * 128], rhs=tab[:],
                                                    start=True, stop=True),
                      reads=["cbt", "tab"], writes=["pmb"])
                for qb_ in range(4):
                    A("dve", lambda e, u=u, qb_=qb_: e.tensor_tensor(
                        out=MB[:, u, :, qb_ * 32:(qb_ + 1) * 32],
                        in0=pmb[:, qb_ * 32:(qb_ + 1) * 32, 0:12].rearrange("s q h -> s h q"),
                        in1=negt[:, u, qb_ * 32:(qb_ + 1) * 32].unsqueeze(1).to_broadcast([128, 12, 32]), op=ALU.add),
                      reads=["pmb", "negt"], writes=["MB"])
            if os.environ.get("MK_DEBUG"):
                mbo = nc.dram_tensor("mbo", [128, 7 * 12 * 128], BF16, kind="ExternalOutput").ap()
                A("sp", lambda e: e.dma_start(out=mbo, in_=MB[:].rearrange("p a b c -> p (a b c)")), reads=["MB"], dma=True)
            S.emit()

        if STAGE >= 1:
            phase1_cache(nc, S, locals())
        if STAGE >= 2:
            phase2_proj(nc, S, locals())
        if STAGE >= 3:
            phase3_attn(nc, S, locals())
        if STAGE >= 4:
            phase4_moe(nc, S, locals())
    return nc


def phase1_cache(nc, S, G):
    A = S.add
    ident_b = G["ident_b"]
    cak, cav, cki, cbk, cbv = G["cak"], G["cav"], G["cki"], G["cbk"], G["cbv"]
    KAT, VA, KIT, KBT, VB = G["KAT"], G["VA"], G["KIT"], G["KBT"], G["VB"]
    with contextlib.ExitStack() as es:
        def sb(name, shape, dt):
            return es.enter_context(nc.sbuf_tensor(name, list(shape), dt))

        def ps(name, shape, dt=F32):
            return es.enter_context(nc.psum_tensor(name, list(shape), dt))

        kin = [sb("kin%d" % i, [128, 4, 1344], BF16) for i in range(2)]
        kout = [sb("kout%d" % i, [128, 11, 512], BF16) for i in range(2)]
        pt = [ps("pt%d" % i, [128, 2, 512], BF16) for i in range(4)]
        rk = Rot("kin", 2)
        ro = Rot("kout", 2)
        rp = Rot("pt", 4)
        for r in range(2):
            ctx = 1 + r
            A("pool", lambda e, r=r, ctx=ctx: e.dma_start(out=VA[ctx][0:PAST, :], in_=cav[r]), writes=[("VA", ctx)], dma=True)
            A("pool", lambda e, r=r, ctx=ctx: e.dma_start(out=VB[ctx][0:PAST, :], in_=cbv[r]), writes=[("VB", ctx)], dma=True)
            for blk in range(8):
                i, kk = rk.next()
                t0 = blk * 512
                A("pool", lambda e, i=i, r=r, t0=t0: e.dma_start(
                    out=kin[i][:, :, 0:256], in_=cak[r, t0:t0 + 512, :].rearrange("(j p) c -> p j c", p=128)),
                  writes=[kk], dma=True)
                A("pool", lambda e, i=i, r=r, t0=t0: e.dma_start(
                    out=kin[i][:, :, 256:320], in_=cki[r, t0:t0 + 512, :].rearrange("(j p) c -> p j c", p=128)),
                  writes=[kk], dma=True)
                A("pool", lambda e, i=i, r=r, t0=t0: e.dma_start(
                    out=kin[i][:, :, 320:1344], in_=cbk[r, t0:t0 + 512, :].rearrange("(j p) c -> p j c", p=128)),
                  writes=[kk], dma=True)
                o, ok = ro.next()
                cts = [(0, 128), (128, 128), (256, 64)] + [(320 + 128 * h, 128) for h in range(8)]
                for pair in range(6):
                    pi, pk = rp.next()
                    sub = cts[2 * pair:2 * pair + 2]
                    for si, (c0, rows) in enumerate(sub):
                        for j in range(4):
                            A("pe", lambda e, pi=pi, si=si, j=j, i=i, c0=c0, rows=rows: e.transpose(
                                pt[pi][0:rows, si, j * 128:(j + 1) * 128], kin[i][:, j, c0:c0 + rows], ident_b[:]),
                              reads=[kk, "ident_b"], writes=[pk])
                    if len(sub) == 2 and sub[1][1] == 128 and sub[0][1] == 128:
                        eng = "act" if pair % 2 == 0 else "dve"
                        if eng == "act":
                            A("act", lambda e, pi=pi, o=o, pair=pair: e.activation(
                                out=kout[o][:, 2 * pair:2 * pair + 2, :], in_=pt[pi][:, :, :], func=AF.Copy),
                              reads=[pk], writes=[ok])
                        else:
                            A("dve", lambda e, pi=pi, o=o, pair=pair: e.tensor_copy(
                                kout[o][:, 2 * pair:2 * pair + 2, :], pt[pi][:, :, :]),
                              reads=[pk], writes=[ok])
                    else:
                        for si, (c0, rows) in enumerate(sub):
                            A("dve", lambda e, pi=pi, o=o, pair=pair, si=si, rows=rows: e.tensor_copy(
                                kout[o][0:rows, 2 * pair + si, :], pt[pi][0:rows, si, :]),
                              reads=[pk], writes=[ok])
                A("sp", lambda e, o=o, ctx=ctx, t0=t0: e.dma_start(
                    out=KAT[ctx][:, :, t0:t0 + 512].rearrange("g d s -> d g s"), in_=kout[o][:, 0:2, :]),
                  reads=[ok], writes=[("KAT", ctx)], dma=True)
                A("sp", lambda e, o=o, ctx=ctx, t0=t0: e.dma_start(
                    out=KIT[ctx][:, t0:t0 + 512], in_=kout[o][0:64, 2, :]),
                  reads=[ok], writes=[("KIT", ctx)], dma=True)
                A("sp", lambda e, o=o, ctx=ctx, t0=t0: e.dma_start(
                    out=KBT[ctx][:, :, t0:t0 + 512].rearrange("g d s -> d g s"), in_=kout[o][:, 3:11, :]),
                  reads=[ok], writes=[("KBT", ctx)], dma=True)
        S.emit()


def phase2_proj(nc, S, G):
    A = S.add
    ident_b = G["ident_b"]
    AaT, BaT = G["AaT"], G["BaT"]
    w_in = G["w_in"]
    KAT, VA, KIT, KBT, VB = G["KAT"], G["VA"], G["KIT"], G["KBT"], G["VB"]
    QAT, QBT, QIT, IWS = G["QAT"], G["QBT"], G["QIT"], G["IWS"]
    xb, xown, xsmp = G["xb"], G["xown"], G["xsmp"]
    wv = w_in.rearrange("(kt p) n -> p kt n", p=128)
    SC = 128 ** -0.5
    with contextlib.ExitStack() as es:
        def sb(name, shape, dt):
            return es.enter_context(nc.sbuf_tensor(name, list(shape), dt))

        def ps(name, shape, dt=F32):
            return es.enter_context(nc.psum_tensor(name, list(shape), dt))

        hT = sb("hT", [128, 16, 2080], BF16)
        xt = [sb("xt%d" % i, [128, D], F32) for i in range(2)]
        xn = [sb("xn%d" % i, [128, D], BF16) for i in range(2)]
        junk = sb("junk", [128, D], BF16)
        ss = [sb("ss%d" % i, [128, 2], F32) for i in range(2)]
        tmpf = [sb("tmpf%d" % i, [128, 8, 128], F32) for i in range(2)]
        wbuf = [sb("wbuf%d" % i, [128, 16, 512], BF16) for i in range(2)]
        stb = [sb("stb%d" % i, [128, 512], BF16) for i in range(3)]
        stf = [sb("stf%d" % i, [128, 512], F32) for i in range(3)]
        pth = [ps("pth%d" % i, [128, 8, 128], BF16) for i in range(2)]
        pf = [ps("pf%d" % i, [128, 512]) for i in range(4)]
        rx, rpt, rtm, rw, rsb, rsf, rpf = (Rot("xt", 2), Rot("pth", 2), Rot("tmpf", 2), Rot("wbuf", 2), Rot("stb", 3),
                                           Rot("stf", 3), Rot("pf", 4))
        cnt = [0]

        def make_hT(xsrc, n, off, segs):
            i, xk = rx.next()
            A("sp", lambda e: e.dma_start(out=xt[i][0:n, :], in_=xsrc), writes=[xk], dma=True)
            A("act", lambda e: e.activation(out=junk[0:n, :], in_=xt[i][0:n, :], func=AF.Square, accum_out=ss[i][0:n, 0:1]),
              reads=[xk], writes=["junk", ("ss", i)])
            A("dve", lambda e: e.tensor_scalar(out=ss[i][0:n, 1:2], in0=ss[i][0:n, 0:1], scalar1=1.0 / D, scalar2=EPS,
                                               op0=ALU.mult, op1=ALU.add), reads=[("ss", i)], writes=[("rs", i)])
            A("act", lambda e: e.activation(out=ss[i][0:n, 1:2], in_=ss[i][0:n, 1:2], func=AF.Sqrt),
              reads=[("rs", i)], writes=[("rs", i)])
            A("dve", lambda e: e.reciprocal(out=ss[i][0:n, 1:2], in_=ss[i][0:n, 1:2]),
              reads=[("rs", i)], writes=[("rs", i)])
            A("dve", lambda e: e.tensor_scalar(out=xn[i][0:n, :], in0=xt[i][0:n, :], scalar1=ss[i][0:n, 1:2], scalar2=None,
                                               op0=ALU.mult), reads=[xk, ("rs", i)], writes=[("xn", i)])
            for half in range(2):
                pi, pk = rpt.next()
                for k8 in range(8):
                    kt = half * 8 + k8
                    A("pe", lambda e, pi=pi, k8=k8, kt=kt: e.transpose(
                        pth[pi][:, k8, 0:n], xn[i][0:n, kt * 128:(kt + 1) * 128], ident_b[0:n, 0:n]),
                      reads=[("xn", i), "ident_b"], writes=[pk])
                ti, tk = rtm.next()
                for (lo, hi, r) in segs:
                    w = hi - lo
                    A("dve", lambda e, pi=pi, ti=ti, lo=lo, hi=hi, r=r, w=w, half=half: e.tensor_tensor(
                        out=tmpf[ti][:, :, lo:hi], in0=pth[pi][:, :, lo:hi],
                        in1=AaT[:, r, half * 8:half * 8 + 8].unsqueeze(2).to_broadcast([128, 8, w]), op=ALU.mult),
                      reads=[pk, "AaT"], writes=[tk])
                    A("pool", lambda e, ti=ti, lo=lo, hi=hi, r=r, w=w, half=half: e.tensor_tensor(
                        out=hT[:, half * 8:half * 8 + 8, off + lo:off + hi], in0=tmpf[ti][:, :, lo:hi],
                        in1=BaT[:, r, half * 8:half * 8 + 8].unsqueeze(2).to_broadcast([128, 8, w]), op=ALU.add),
                      reads=[tk, "BaT"], writes=[("hT", off)])

        def load_w(c0, ncb):
            i, wk = rw.next()
            A("pool", lambda e: e.dma_start(out=wbuf[i][:, :, 0:ncb], in_=wv[:, :, c0:c0 + ncb]), writes=[wk], dma=True)
            return i, wk

        def evac(dst, src, scale, idx):
            if idx % 2 == 0:
                if scale == 1.0:
                    return ("act", lambda e: e.activation(out=dst, in_=src, func=AF.Copy))
                return ("act", lambda e: e.mul(dst, src, scale))
            if scale == 1.0:
                return ("dve", lambda e: e.tensor_copy(dst, src))
            return ("dve", lambda e: e.tensor_scalar(out=dst, in0=src, scalar1=scale, scalar2=None, op0=ALU.mult))

        def do_fm(i, wk, ncb, ct_base, chunks, scale, hkeys, outfn):
            for ct in range((ncb + 127) // 128):
                rows = min(128, ncb - ct * 128)
                for (t0, nt) in chunks:
                    pi, pk = rpf.next()
                    for kt in range(16):
                        A("pe", lambda e, pi=pi, kt=kt, ct=ct, rows=rows, t0=t0, nt=nt: e.matmul(
                            pf[pi][0:rows, 0:nt], lhsT=wbuf[i][:, kt, ct * 128:ct * 128 + rows], rhs=hT[:, kt, t0:t0 + nt],
                            start=(kt == 0), stop=(kt == 15)), reads=[wk] + hkeys, writes=[pk])
                    si, sk = rsb.next()
                    cnt[0] += 1
                    eng, fn = evac(stb[si][0:rows, 0:nt], pf[pi][0:rows, 0:nt], scale, cnt[0])
                    A(eng, fn, reads=[pk], writes=[sk])
                    outfn(stb[si], sk, rows, ct_base + ct, t0, nt)

        def do_tm(i, wk, ncb, cb0, tiles, scale, outfn):
            for (tix, off, n) in tiles:
                pi, pk = rpf.next()
                for kt in range(16):
                    A("pe", lambda e, pi=pi, kt=kt, off=off, n=n: e.matmul(
                        pf[pi][0:n, 0:ncb], lhsT=hT[:, kt, off:off + n], rhs=wbuf[i][:, kt, 0:ncb],
                        start=(kt == 0), stop=(kt == 15)), reads=[wk, ("hT", off)], writes=[pk])
                si, sk = rsf.next()
                cnt[0] += 1
                eng, fn = evac(stf[si][0:n, 0:ncb], pf[pi][0:n, 0:ncb], scale, cnt[0])
                A(eng, fn, reads=[pk], writes=[sk])
                outfn(stf[si], sk, tix, n, cb0, ncb)

        def dma_out(dst, src, sk, wkey, q="sp"):
            A(q, lambda e: e.dma_start(out=dst, in_=src), reads=[sk], writes=[wkey], dma=True)

        for ch in range(4):
            T0 = ch * 2048
            for t in range(16):
                make_hT(xb[T0 + t * 128:T0 + (t + 1) * 128, :], 128, t * 128, [(0, 128, 0)])
            hkeys = [("hT", t * 128) for t in range(16)]
            chunks = [(t0, 512) for t0 in range(0, 2048, 512)]
            tiles = [(t, t * 128, 128) for t in range(16)]

            def fm_out(dstT):
                def f(st, sk, rows, ctg, t0, nt):
                    dma_out(dstT[ctg][0:rows, T0 + t0:T0 + t0 + nt], st[0:rows, 0:nt], sk, ("scrK", id(dstT)))
                return f

            def tm_out(dstT):
                def f(st, sk, tix, n, cb0, ncb):
                    dma_out(dstT[T0 + tix * 128:T0 + tix * 128 + n, cb0:cb0 + ncb], st[0:n, 0:ncb], sk,
                            ("scrV", id(dstT)), q="pool")
                return f
            i, wk = load_w(1024, 256)
            do_fm(i, wk, 256, 0, chunks, 1.0, hkeys, fm_out(KAT[0]))
            i, wk = load_w(2560, 64)
            do_fm(i, wk, 64, 0, chunks, 1.0, hkeys, lambda st, sk, rows, ctg, t0, nt: dma_out(
                KIT[0][0:64, T0 + t0:T0 + t0 + nt], st[0:64, 0:nt], sk, "scrKI"))
            for cbk_ in range(2):
                i, wk = load_w(3664 + 512 * cbk_, 512)
                do_fm(i, wk, 512, 4 * cbk_, chunks, 1.0, hkeys, fm_out(KBT[0]))
            i, wk = load_w(1280, 256)
            do_tm(i, wk, 256, 0, tiles, 1.0, tm_out(VA[0]))
            for cbv_ in range(2):
                i, wk = load_w(4688 + 512 * cbv_, 512)
                do_tm(i, wk, 512, 512 * cbv_, tiles, 1.0, tm_out(VB[0]))

        for t in range(16):
            make_hT(xown[t * 128:(t + 1) * 128, :], 128, t * 128, [(0, 128, 0)])
        make_hT(xsmp, 32, 2048, [(0, 16, 1), (16, 32, 2)])
        hkeys = [("hT", t * 128) for t in range(17)]
        chunks = [(t0, 512) for t0 in range(0, 2048, 512)] + [(2048, 32)]
        tiles = [(t, t * 128, 128) for t in range(16)] + [(16, 2048, 32)]
        schunk = [(2048, 32)]
        G["p2"] = dict(ak=(G["ak_own"], G["ak_s"]), av=(G["av_own"], G["av_s"]), ki=(G["ki_own"], G["ki_s"]),
                       bk=(G["bk_own"], G["bk_s"]), bv=(G["bv_own"], G["bv_s"]))

        def q_out(dstT):
            def f(st, sk, rows, ctg, t0, nt):
                dma_out(dstT[ctg][0:rows, t0:t0 + nt], st[0:rows, 0:nt], sk, ("scrQ", id(dstT)))
            return f

        def rows_out(name, vscr=None):
            own, smp = G["p2"][name]

            def f(st, sk, tix, n, cb0, ncb):
                if tix < 16:
                    dma_out(own[tix * 128:(tix + 1) * 128, cb0:cb0 + ncb], st[0:n, 0:ncb], sk, ("out", name))
                else:
                    dma_out(smp[:, cb0:cb0 + ncb], st[0:n, 0:ncb], sk, ("out", name))
                    if vscr is not None:
                        for r in range(2):
                            dma_out(vscr[1 + r][PAST:PAST + 16, cb0:cb0 + ncb], st[16 * r:16 * r + 16, 0:ncb], sk,
                                    ("scrVs", name, r), q="pool")
            return f

        def sk_out(dstT):
            def f(st, sk, rows, ctg, t0, nt):
                for r in range(2):
                    dma_out(dstT[1 + r][ctg][0:rows, PAST:PAST + 16], st[0:rows, 16 * r:16 * r + 16], sk, ("scrKs", id(dstT), r))
            return f
        for cb in range(2):
            i, wk = load_w(512 * cb, 512)
            do_fm(i, wk, 512, 4 * cb, chunks, SC, hkeys, q_out(QAT))
        i, wk = load_w(1024, 256)
        do_tm(i, wk, 256, 0, tiles, 1.0, rows_out("ak"))
        do_fm(i, wk, 256, 0, schunk, 1.0, hkeys, lambda st, sk, rows, ctg, t0, nt: [dma_out(
            KAT[1 + r][ctg][0:rows, PAST:PAST + 16], st[0:rows, 16 * r:16 * r + 16], sk, ("scrKs", "a", r)) for r in range(2)])
        i, wk = load_w(1280, 256)
        do_tm(i, wk, 256, 0, tiles, 1.0, rows_out("av", VA))
        for cb in range(2):
            i, wk = load_w(1536 + 512 * cb, 512)
            do_fm(i, wk, 512, 4 * cb, chunks, 0.125, hkeys, q_out(QIT))
        i, wk = load_w(2560, 64)
        do_tm(i, wk, 64, 0, tiles, 1.0, rows_out("ki"))
        do_fm(i, wk, 64, 0, schunk, 1.0, hkeys, lambda st, sk, rows, ctg, t0, nt: [dma_out(
            KIT[1 + r][0:64, PAST:PAST + 16], st[0:64, 16 * r:16 * r + 16], sk, ("scrKs", "i", r)) for r in range(2)])
        i, wk = load_w(2624, 16)
        do_tm(i, wk, 16, 0, tiles, 0.25, lambda st, sk, tix, n, cb0, ncb: dma_out(
            IWS[tix * 128:tix * 128 + n, :], st[0:n, 0:16], sk, "scrIW"))
        for cb in range(2):
            i, wk = load_w(2640 + 512 * cb, 512)
            do_fm(i, wk, 512, 4 * cb, chunks, SC, hkeys, q_out(QBT))
        for cb in range(2):
            i, wk = load_w(3664 + 512 * cb, 512)
            do_tm(i, wk, 512, 512 * cb, tiles, 1.0, rows_out("bk"))
            do_fm(i, wk, 512, 4 * cb, schunk, 1.0, hkeys, lambda st, sk, rows, ctg, t0, nt: [dma_out(
                KBT[1 + r][ctg][0:rows, PAST:PAST + 16], st[0:rows, 16 * r:16 * r + 16], sk, ("scrKs", "b", r)) for r in range(2)])
        for cb in range(2):
            i, wk = load_w(4688 + 512 * cb, 512)
            do_tm(i, wk, 512, 512 * cb, tiles, 1.0, rows_out("bv", VB))
        S.emit()


def phase3_attn(nc, S, G):
    A = S.add
    ident_b, ones_b, MB, lamt, sg8 = G["ident_b"], G["ones_b"], G["MB"], G["lamt"], G["sg8"]
    KAT, VA, KIT, KBT, VB = G["KAT"], G["VA"], G["KIT"], G["KBT"], G["VB"]
    QAT, QBT, QIT, IWS, mixS = G["QAT"], G["QBT"], G["QIT"], G["IWS"], G["mixS"]
    vis01, visneg = G["vis01"], G["visneg"]
    with contextlib.ExitStack() as es:
        def sb(name, shape, dt):
            return es.enter_context(nc.sbuf_tensor(name, list(shape), dt))

        def ps(name, shape, dt=F32):
            return es.enter_context(nc.psum_tensor(name, list(shape), dt))

        score = sb("score", [128, SEQ], F32)
        maskb = sb("maskb", [128, SEQ], BF16)
        v01 = sb("v01", [128, 512], F32)
        vng = sb("vng", [128, 512], F32)
        kilo = [sb("kilo%d" % i, [128, 512], BF16) for i in range(2)]
        kihi = [sb("kihi%d" % i, [128, 512], BF16) for i in range(2)]
        rbuf = [sb("rbuf%d" % i, [128, 512], BF16) for i in range(3)]
        kab = [sb("kab%d" % i, [128, 2, 512], BF16) for i in range(2)]
        vab = [sb("vab%d" % i, [128, 4, 256], BF16) for i in range(2)]
        kbb = [sb("kbb%d" % i, [128, 8, 512], BF16) for i in range(2)]
        vbb = [sb("vbb%d" % i, [128, 4, 1024], BF16) for i in range(2)]
        qa = [sb("qa%d" % i, [128, 8, 128], BF16) for i in range(2)]
        qb = [sb("qb%d" % i, [128, 8, 128], BF16) for i in range(2)]
        qi = [sb("qi%d" % i, [128, 8, 128], BF16) for i in range(2)]
        iw = [sb("iw%d" % i, [128, 16], F32) for i in range(2)]
        pbuf = [sb("pbuf%d" % i, [128, 4, 128], BF16) for i in range(4)]
        mix = [sb("mix%d" % i, [128, D], BF16) for i in range(2)]
        ob = sb("ob", [128, 4, 256], F32)
        junk = sb("junk3", [128, 256], BF16)
        bis = sb("bis", [128, 8], F32)
        zr = sb("zr", [128, 24], F32)
        psS = [ps("psS%d" % i, [128, 4, 128]) for i in range(3)]
        psO = [ps("psO%d" % i, [128, 512]) for i in range(4)]
        psZ = ps("psZ", [128, 16])
        rki, rrb, rka, rkb, rpb, rpS = Rot("ki", 2), Rot("rbuf", 3), Rot("ka", 2), Rot("kb", 2), Rot("pbuf", 4), Rot("psS", 3)

        A("sp", lambda e: e.dma_start(out=v01[:], in_=vis01), writes=["v01"], dma=True)
        A("sp", lambda e: e.dma_start(out=vng[:], in_=visneg), writes=["vng"], dma=True)
        for i in range(2):
            A("pool", lambda e, i=i: e.memset(kilo[i][:], 0.0), writes=[("ki", i)])
            A("pool", lambda e, i=i: e.memset(kihi[i][:], 0.0), writes=[("ki", i)])

        for n in range(18):
            if n < 16:
                m, ctx, nq, NT, tok0, lastk = n, 0, 128, 4 * n + 4, n * 128, 128
                slot_of = lambda t, m=m: (t - (4 * m - 1)) if t >= 4 * m - 1 else None
            else:
                r = n - 16
                m, ctx, nq, NT, tok0, lastk = None, 1 + r, 16, 33, 2048 + 16 * r, 16
                slot_of = lambda t: {31: 5, 32: 6}.get(t)
            L = (NT - 1) * 128 + lastk
            qs = n % 2
            qk = ("q", qs)
            A("sp", lambda e, qs=qs, tok0=tok0, nq=nq: e.dma_start(
                out=qa[qs][:, :, 0:nq], in_=QAT[:, :, tok0:tok0 + nq].rearrange("h d q -> d h q")),
              reads=[("scrQ", id(QAT))], writes=[qk], dma=True)
            A("sp", lambda e, qs=qs, tok0=tok0, nq=nq: e.dma_start(
                out=qb[qs][:, :, 0:nq], in_=QBT[:, :, tok0:tok0 + nq].rearrange("h d q -> d h q")),
              writes=[qk], dma=True)
            A("sp", lambda e, qs=qs, tok0=tok0, nq=nq: e.dma_start(
                out=qi[qs][:, :, 0:nq], in_=QIT[:, :, tok0:tok0 + nq].rearrange("h d q -> d h q")),
              writes=[qk], dma=True)
            A("sp", lambda e, qs=qs, tok0=tok0, nq=nq: e.dma_start(out=iw[qs][0:nq, :], in_=IWS[tok0:tok0 + nq, :]),
              writes=[qk], dma=True)
            blocks = []
            for k0 in range(0, L, 512):
                blocks.append((k0, min(512, L - k0)))
            for (k0, nk) in blocks:
                i, kk = rki.next()
                A("sp", lambda e, i=i, k0=k0, nk=nk, ctx=ctx: e.dma_start(out=kilo[i][0:64, 0:nk], in_=KIT[ctx][:, k0:k0 + nk]),
                  writes=[kk], dma=True)
                A("sp", lambda e, i=i, k0=k0, nk=nk, ctx=ctx: e.dma_start(out=kihi[i][64:128, 0:nk], in_=KIT[ctx][:, k0:k0 + nk]),
                  writes=[kk], dma=True)
                for h in range(16):
                    pi, pk = rpS.next()
                    src = kilo if h % 2 == 0 else kihi
                    pflat = psS[pi][:].rearrange("p a b -> p (a b)")
                    A("pe", lambda e, pflat=pflat, src=src, i=i, h=h, nk=nk, qs=qs, nq=nq: e.matmul(
                        pflat[0:nq, 0:nk], lhsT=qi[qs][:, h // 2, 0:nq], rhs=src[i][:, 0:nk], start=True, stop=True),
                      reads=[qk, kk], writes=[pk])
                    ri, rk = rrb.next()
                    A("act", lambda e, pflat=pflat, ri=ri, nk=nk, nq=nq: e.activation(
                        out=rbuf[ri][0:nq, 0:nk], in_=pflat[0:nq, 0:nk], func=AF.Relu), reads=[pk], writes=[rk])
                    if h == 0:
                        A("dve", lambda e, ri=ri, k0=k0, nk=nk, nq=nq, qs=qs: e.tensor_scalar(
                            out=score[0:nq, k0:k0 + nk], in0=rbuf[ri][0:nq, 0:nk], scalar1=iw[qs][0:nq, 0:1], scalar2=None,
                            op0=ALU.mult), reads=[rk, qk], writes=["score"])
                    else:
                        A("dve", lambda e, ri=ri, k0=k0, nk=nk, nq=nq, qs=qs, h=h: e.scalar_tensor_tensor(
                            out=score[0:nq, k0:k0 + nk], in0=rbuf[ri][0:nq, 0:nk], scalar=iw[qs][0:nq, h:h + 1],
                            in1=score[0:nq, k0:k0 + nk], op0=ALU.mult, op1=ALU.add), reads=[rk, qk, "score"], writes=["score"])
            if n < 16:
                k0 = 4 * m * 128
                A("dve", lambda e, k0=k0: e.tensor_tensor(out=score[:, k0:k0 + 512], in0=score[:, k0:k0 + 512], in1=v01[:],
                                                          op=ALU.mult), reads=["score", "v01"], writes=["score"])
            A("dve", lambda e, nq=nq, L=L: e.tensor_reduce(out=bis[0:nq, 0:1], in_=score[0:nq, 0:L], axis=AX.X, op=ALU.max,
                                                           apply_absolute_value=True), reads=["score"], writes=["bisA"])
            if n < 16:
                A("dve", lambda e, k0=k0: e.tensor_tensor(out=score[:, k0:k0 + 512], in0=score[:, k0:k0 + 512], in1=vng[:],
                                                          op=ALU.add), reads=["score", "vng", "bisA"], writes=["score"])
            A("dve", lambda e, nq=nq: e.tensor_scalar(out=bis[0:nq, 1:2], in0=bis[0:nq, 0:1], scalar1=2.0, scalar2=None,
                                                      op0=ALU.mult), reads=["bisA"], writes=["bisR"])
            A("dve", lambda e, nq=nq: e.tensor_scalar(out=bis[0:nq, 2:3], in0=bis[0:nq, 0:1], scalar1=-1.0, scalar2=None,
                                                      op0=ALU.mult), reads=["bisA"], writes=["bisT"])
            for it in range(NBIS):
                c = 2.0 ** -(it + 1)
                A("dve", lambda e, nq=nq, c=c: e.scalar_tensor_tensor(
                    out=bis[0:nq, 3:4], in0=bis[0:nq, 1:2], scalar=c, in1=bis[0:nq, 2:3], op0=ALU.mult, op1=ALU.add),
                  reads=["bisR", "bisT"], writes=["bisC"])
                A("dve", lambda e, nq=nq, L=L: e.tensor_scalar(
                    out=maskb[0:nq, 0:L], in0=score[0:nq, 0:L], scalar1=bis[0:nq, 3:4], scalar2=None, op0=ALU.is_ge,
                    op1=ALU.add, accum_out=bis[0:nq, 4:5]), reads=["score", "bisC"], writes=["maskb", "bisN"])
                A("dve", lambda e, nq=nq, c=c: e.tensor_scalar(
                    out=bis[0:nq, 5:6], in0=bis[0:nq, 4:5], scalar1=255.5, scalar2=c, op0=ALU.is_ge, op1=ALU.mult),
                  reads=["bisN"], writes=["bisM"])
                A("dve", lambda e, nq=nq: e.scalar_tensor_tensor(
                    out=bis[0:nq, 2:3], in0=bis[0:nq, 5:6], scalar=bis[0:nq, 1:2], in1=bis[0:nq, 2:3], op0=ALU.mult,
                    op1=ALU.add), reads=["bisM", "bisR", "bisT"], writes=["bisT"])
            A("dve", lambda e, nq=nq, L=L: e.tensor_scalar(
                out=maskb[0:nq, 0:L], in0=score[0:nq, 0:L], scalar1=bis[0:nq, 2:3], scalar2=NEGV, op0=ALU.is_lt, op1=ALU.mult),
              reads=["score", "bisT"], writes=["maskb"])
            tiles = [(t, 128 if t < NT - 1 else lastk) for t in range(NT)]
            if n < 16 and m == 0:
                pass
            t_first, t_last = 0, NT - 1
            psOA = [psO[b][:].rearrange("p (a c) -> p a c", a=4) for b in range(2)]
            psOB = [psO[b][:].rearrange("p (a c) -> p a c", a=2) for b in range(4)]
            for (k0, nk) in blocks:
                i, kk = rka.next()
                A("sp", lambda e, i=i, k0=k0, nk=nk, ctx=ctx: e.dma_start(
                    out=kab[i][:, :, 0:nk], in_=KAT[ctx][:, :, k0:k0 + nk].rearrange("g d s -> d g s")), writes=[kk], dma=True)
                if nk == 512:
                    A("sp", lambda e, i=i, k0=k0, ctx=ctx: e.dma_start(
                        out=vab[i][:], in_=VA[ctx][k0:k0 + 512, :].rearrange("(j p) c -> p j c", p=128)), writes=[kk], dma=True)
                else:
                    A("sp", lambda e, i=i, k0=k0, nk=nk, ctx=ctx: e.dma_start(
                        out=vab[i][0:nk, 0, :], in_=VA[ctx][k0:k0 + nk, :]), writes=[kk], dma=True)
                for j in range((nk + 127) // 128):
                    t = k0 // 128 + j
                    ns = min(128, nk - j * 128)
                    slot = slot_of(t)
                    for g in range(2):
                        pi, pk = rpS.next()
                        A("pe", lambda e, pi=pi, i=i, g=g, j=j, ns=ns, qs=qs, nq=nq: e.matmul(
                            psS[pi][0:ns, :, 0:nq], lhsT=kab[i][:, g, j * 128:j * 128 + ns], rhs=qa[qs][:, 4 * g:4 * g + 4, 0:nq],
                            start=True, stop=False), reads=[kk, qk], writes=[pk])
                        A("pe", lambda e, pi=pi, t=t, ns=ns, nq=nq, slot=slot: e.matmul(
                            psS[pi][0:ns, :, 0:nq], lhsT=maskb[0:nq, t * 128:t * 128 + ns],
                            rhs=ident_b[0:nq, 0:nq].unsqueeze(1).to_broadcast([nq, 4, nq]),
                            start=False, stop=(slot is None)), reads=["maskb", "ident_b"], writes=[pk])
                        if slot is not None:
                            A("pe", lambda e, pi=pi, ns=ns, nq=nq, slot=slot, g=g: e.matmul(
                                psS[pi][0:ns, :, 0:nq], lhsT=ident_b[0:ns, 0:ns], rhs=MB[0:ns, slot, 4 * g:4 * g + 4, 0:nq],
                                start=False, stop=True), reads=["MB", "ident_b"], writes=[pk])
                        bi, bk = rpb.next()
                        A("act", lambda e, pi=pi, bi=bi, ns=ns, nq=nq: e.activation(
                            out=pbuf[bi][0:ns, :, 0:nq], in_=psS[pi][0:ns, :, 0:nq], func=AF.Exp), reads=[pk], writes=[bk])
                        for hh in range(4):
                            h = 4 * g + hh
                            A("pe", lambda e, bi=bi, hh=hh, h=h, ns=ns, nq=nq, i=i, j=j, g=g, t=t: e.matmul(
                                psOA[h // 4][0:nq, h % 4, :], lhsT=pbuf[bi][0:ns, hh, 0:nq], rhs=vab[i][0:ns, j, g * 128:(g + 1) * 128],
                                start=(t == t_first and hh == 0), stop=(t == t_last)), reads=[bk, kk], writes=[("psO", h // 4)])
                            A("pe", lambda e, bi=bi, hh=hh, h=h, ns=ns, nq=nq, t=t: e.matmul(
                                psZ[0:nq, h:h + 1], lhsT=pbuf[bi][0:ns, hh, 0:nq], rhs=ones_b[0:ns, 0:1],
                                start=(t == t_first and h == 0), stop=(t == t_last)), reads=[bk, "ones_b"], writes=["psZ"])
            mi = n % 2
            mk = ("mix", mi)
            A("dve", lambda e, nq=nq: e.reciprocal(out=zr[0:nq, 0:8], in_=psZ[0:nq, 0:8]), reads=["psZ"], writes=["zrA"])
            for h in range(8):
                if h % 2 == 0:
                    A("dve", lambda e, h=h, nq=nq, mi=mi: e.tensor_scalar(
                        out=mix[mi][0:nq, h * 128:(h + 1) * 128], in0=psOA[h // 4][0:nq, h % 4, :], scalar1=zr[0:nq, h:h + 1],
                        scalar2=None, op0=ALU.mult), reads=[("psO", h // 4), "zrA"], writes=[mk])
                else:
                    A("act", lambda e, h=h, nq=nq, mi=mi: e.activation(
                        out=mix[mi][0:nq, h * 128:(h + 1) * 128], in_=psOA[h // 4][0:nq, h % 4, :], func=AF.Copy,
                        scale=zr[0:nq, h:h + 1]), reads=[("psO", h // 4), "zrA"], writes=[mk])
            for (k0, nk) in blocks:
                i, kk = rkb.next()
                A("sp", lambda e, i=i, k0=k0, nk=nk, ctx=ctx: e.dma_start(
                    out=kbb[i][:, :, 0:nk], in_=KBT[ctx][:, :, k0:k0 + nk].rearrange("g d s -> d g s")), writes=[kk], dma=True)
                if nk == 512:
                    A("sp", lambda e, i=i, k0=k0, ctx=ctx: e.dma_start(
                        out=vbb[i][:], in_=VB[ctx][k0:k0 + 512, :].rearrange("(j p) c -> p j c", p=128)), writes=[kk], dma=True)
                else:
                    A("sp", lambda e, i=i, k0=k0, nk=nk, ctx=ctx: e.dma_start(
                        out=vbb[i][0:nk, 0, :], in_=VB[ctx][k0:k0 + nk, :]), writes=[kk], dma=True)
                for j in range((nk + 127) // 128):
                    t = k0 // 128 + j
                    ns = min(128, nk - j * 128)
                    slot = slot_of(t)
                    for half in range(2):
                        pi, pk = rpS.next()
                        for q4 in range(4):
                            hc = half * 4 + q4
                            h = hc // 2
                            A("pe", lambda e, pi=pi, i=i, hc=hc, q4=q4, j=j, ns=ns, qs=qs, nq=nq, slot=slot: e.matmul(
                                psS[pi][0:ns, q4, 0:nq], lhsT=kbb[i][:, hc, j * 128:j * 128 + ns], rhs=qb[qs][:, hc, 0:nq],
                                start=True, stop=(slot is None)), reads=[kk, qk], writes=[pk])
                            if slot is not None:
                                A("pe", lambda e, pi=pi, q4=q4, ns=ns, nq=nq, slot=slot, h=h: e.matmul(
                                    psS[pi][0:ns, q4, 0:nq], lhsT=ident_b[0:ns, 0:ns], rhs=MB[0:ns, slot, 8 + h, 0:nq],
                                    start=False, stop=True), reads=["MB", "ident_b"], writes=[pk])
                        bi, bk = rpb.next()
                        A("act", lambda e, pi=pi, bi=bi, ns=ns, nq=nq: e.activation(
                            out=pbuf[bi][0:ns, :, 0:nq], in_=psS[pi][0:ns, :, 0:nq], func=AF.Exp), reads=[pk], writes=[bk])
                        for q4 in range(4):
                            hc = half * 4 + q4
                            h = hc // 2
                            A("pe", lambda e, bi=bi, q4=q4, hc=hc, h=h, ns=ns, nq=nq, i=i, j=j, t=t: e.matmul(
                                psOB[hc // 2][0:nq, hc % 2, :], lhsT=pbuf[bi][0:ns, q4, 0:nq], rhs=vbb[i][0:ns, j, h * 256:(h + 1) * 256],
                                start=(t == t_first and hc % 2 == 0), stop=(t == t_last)), reads=[bk, kk], writes=[("psO", hc // 2)])
                            A("pe", lambda e, bi=bi, q4=q4, hc=hc, ns=ns, nq=nq, t=t: e.matmul(
                                psZ[0:nq, 8 + hc:9 + hc], lhsT=pbuf[bi][0:ns, q4, 0:nq], rhs=ones_b[0:ns, 0:1],
                                start=(t == t_first and hc == 0), stop=(t == t_last)), reads=[bk, "ones_b"], writes=["psZ"])
            A("dve", lambda e, nq=nq: e.reciprocal(out=zr[0:nq, 8:16], in_=psZ[0:nq, 8:16]), reads=["psZ"], writes=["zrB"])
            for h in range(4):
                A("dve", lambda e, h=h, nq=nq: e.tensor_scalar(
                    out=zr[0:nq, 16 + h:17 + h], in0=zr[0:nq, 9 + 2 * h:10 + 2 * h], scalar1=lamt[0:nq, 1:2], scalar2=None,
                    op0=ALU.mult), reads=["zrB"], writes=["zrL"])
                A("dve", lambda e, h=h, nq=nq: e.tensor_scalar(
                    out=ob[0:nq, h, :], in0=psOB[h][0:nq, 0, :], scalar1=zr[0:nq, 8 + 2 * h:9 + 2 * h], scalar2=None,
                    op0=ALU.mult), reads=[("psO", h), "zrB"], writes=[("ob", h)])
                A("dve", lambda e, h=h, nq=nq: e.scalar_tensor_tensor(
                    out=ob[0:nq, h, :], in0=psOB[h][0:nq, 1, :], scalar=zr[0:nq, 16 + h:17 + h], in1=ob[0:nq, h, :],
                    op0=ALU.mult, op1=ALU.add), reads=[("psO", h), "zrL", ("ob", h)], writes=[("ob", h)])
                A("act", lambda e, h=h, nq=nq: e.activation(
                    out=junk[0:nq, :], in_=ob[0:nq, h, :], func=AF.Square, accum_out=zr[0:nq, 20 + h:21 + h]),
                  reads=[("ob", h)], writes=["junk3", "zrS"])
            A("dve", lambda e, nq=nq: e.tensor_scalar(out=zr[0:nq, 20:24], in0=zr[0:nq, 20:24], scalar1=1.0 / 256, scalar2=EPS,
                                                      op0=ALU.mult, op1=ALU.add), reads=["zrS"], writes=["zrS"])
            A("act", lambda e, nq=nq: e.activation(out=zr[0:nq, 20:24], in_=zr[0:nq, 20:24], func=AF.Sqrt),
              reads=["zrS"], writes=["zrS"])
            A("dve", lambda e, nq=nq: e.reciprocal(out=zr[0:nq, 20:24], in_=zr[0:nq, 20:24]), reads=["zrS"], writes=["zrS"])
            for h in range(4):
                A("dve", lambda e, h=h, nq=nq, mi=mi: e.scalar_tensor_tensor(
                    out=mix[mi][0:nq, 1024 + h * 256:1024 + (h + 1) * 256], in0=ob[0:nq, h, :], scalar=zr[0:nq, 20 + h:21 + h],
                    in1=sg8[0:nq, :], op0=ALU.mult, op1=ALU.mult), reads=[("ob", h), "zrS", "sg8"], writes=[mk])
            A("sp", lambda e, mi=mi, nq=nq, tok0=tok0: e.dma_start(out=mixS[tok0:tok0 + nq, :], in_=mix[mi][0:nq, :]),
              reads=[mk], writes=["mixS"], dma=True)
        S.emit()


def phase4_moe(nc, S, G):
    A = S.add
    ident_b, ident_f = G["ident_b"], G["ident_f"]
    AfT, BfT, rbias = G["AfT"], G["BfT"], G["rbias"]
    modS, mixS, x1S = G["modS"], G["mixS"], G["x1S"]
    xown, xsmp = G["xown"], G["xsmp"]
    w_out, w_router = G["w_out"], G["w_router"]
    w_gate, w_up, w_down = G["w_gate"], G["w_up"], G["w_down"]
    ws_gate, ws_up, ws_down, final_g = G["ws_gate"], G["ws_up"], G["ws_down"], G["final_g"]
    y_own, y_smp = G["y_own"], G["y_smp"]
    BIG = 1.0e9
    for hf in range(2):
        tiles = []
        for tl in range(8):
            n = hf * 8 + tl
            tiles.append((tl, 128, tl * 128, n * 128, xown[n * 128:(n + 1) * 128, :], [(0, 128, 0)]))
        if hf == 1:
            tiles.append((8, 32, 1024, 2048, xsmp, [(0, 16, 1), (16, 32, 2)]))
        TH = 1024 + (32 if hf == 1 else 0)
        blocks = [(0, 512), (512, 512)] + ([(1024, 32)] if hf == 1 else [])
        with contextlib.ExitStack() as oes:
            h2T = oes.enter_context(nc.sbuf_tensor("h2T_h%d" % hf, [128, 16, 1056], BF16))
            gatesT = oes.enter_context(nc.sbuf_tensor("gatesT_h%d" % hf, [65, 1056], F32))
            with contextlib.ExitStack() as es:
                def sb(name, shape, dt):
                    return es.enter_context(nc.sbuf_tensor(name + "_h%d" % hf, list(shape), dt))

                def ps(name, shape, dt=F32):
                    return es.enter_context(nc.psum_tensor(name + "_h%d" % hf, list(shape), dt))

                wo = sb("wo", [128, 16, D], BF16)
                wr = sb("wr", [128, 16, NEXP], BF16)
                gab = [sb("gab%d" % i, [128, D], F32) for i in range(2)]
                mixt = [sb("mixt%d" % i, [128, D], BF16) for i in range(2)]
                mixT = [sb("mixT%d" % i, [128, 16, 128], BF16) for i in range(2)]
                xt = [sb("xt4%d" % i, [128, D], F32) for i in range(1)]
                x1 = [sb("x1_%d" % i, [128, D], F32) for i in range(1)]
                xn = [sb("xn4%d" % i, [128, D], BF16) for i in range(2)]
                junk = sb("junk4", [128, D], BF16)
                tmpf = [sb("tmpf4%d" % i, [128, 8, 128], F32) for i in range(2)]
                ss = [sb("ss4%d" % i, [128, 2], F32) for i in range(2)]
                rt = [sb("rt%d" % i, [128, 6, NEXP], F32) for i in range(2)]
                rs_ = [sb("rs%d" % i, [128, 48], F32) for i in range(2)]
                pth = [ps("pth4%d" % i, [128, 8, 128], BF16) for i in range(2)]
                py = [ps("py%d" % i, [128, 512]) for i in range(3)]
                pr = ps("pr", [128, NEXP])
                pg = ps("pg", [128, 128])
                rpt, rpy, rtm = Rot("pth4", 2), Rot("py", 3), Rot("tmpf4", 2)
                wov = w_out.rearrange("(ft p) d -> p ft d", p=128)
                for c in range(4):
                    A("pool", lambda e, c=c: e.dma_start(out=wo[:, 4 * c:4 * c + 4, :], in_=wov[:, 4 * c:4 * c + 4, :]),
                      writes=[("wo", c)], dma=True)
                A("pool", lambda e: e.dma_start(out=wr[:], in_=w_router.rearrange("(kt p) n -> p kt n", p=128)),
                  writes=["wr"], dma=True)
                A("sp", lambda e: e.dma_start(out=gab[0][:], in_=modS[0:1, 4096:6144].partition_broadcast(128)),
                  writes=[("gab", 0)], dma=True)
                if hf == 1:
                    for r in range(2):
                        A("sp", lambda e, r=r: e.dma_start(out=gab[1][16 * r:16 * r + 16, :],
                                                           in_=modS[1 + r:2 + r, 4096:6144].partition_broadcast(16)),
                          writes=[("gab", 1)], dma=True)
                A("pool", lambda e: e.memset(gatesT[64:65, :], 1.0), writes=["gatesT"])
                for (tl, nq, off, tok0, xsrc, segs) in tiles:
                    i = tl % 2
                    gi = 1 if nq == 32 else 0
                    A("sp", lambda e, i=i, nq=nq, tok0=tok0: e.dma_start(out=mixt[0][0:nq, :], in_=mixS[tok0:tok0 + nq, :]),
                      reads=["mixS"], writes=[("mixt", i)], dma=True)
                    A("sp", lambda e, i=i, nq=nq, xsrc=xsrc: e.dma_start(out=xt[0][0:nq, :], in_=xsrc), writes=[("xt", 0)], dma=True)
                    for half in range(2):
                        pi, pk = rpt.next()
                        for k8 in range(8):
                            ft = half * 8 + k8
                            A("pe", lambda e, pi=pi, k8=k8, ft=ft, i=i, nq=nq: e.transpose(
                                pth[pi][:, k8, 0:nq], mixt[0][0:nq, ft * 128:(ft + 1) * 128], ident_b[0:nq, 0:nq]),
                              reads=[("mixt", i), "ident_b"], writes=[pk])
                        if half == 0:
                            A("act", lambda e, pi=pi, i=i, nq=nq: e.activation(out=mixT[i][:, 0:8, 0:nq], in_=pth[pi][:, :, 0:nq],
                                                                               func=AF.Copy), reads=[pk], writes=[("mixT", i)])
                        else:
                            A("dve", lambda e, pi=pi, i=i, nq=nq: e.tensor_copy(mixT[i][:, 8:16, 0:nq], pth[pi][:, :, 0:nq]),
                              reads=[pk], writes=[("mixT", i)])
                    for db in range(4):
                        yi, yk = rpy.next()
                        for ft in range(16):
                            A("pe", lambda e, yi=yi, ft=ft, db=db, i=i, nq=nq: e.matmul(
                                py[yi][0:nq, :], lhsT=mixT[i][:, ft, 0:nq], rhs=wo[:, ft, db * 512:(db + 1) * 512],
                                start=(ft == 0), stop=(ft == 15)), reads=[("mixT", i), ("wo", ft // 4)], writes=[yk])
                        A("dve", lambda e, yi=yi, db=db, i=i, nq=nq, gi=gi: e.tensor_tensor(
                            out=x1[0][0:nq, db * 512:(db + 1) * 512], in0=py[yi][0:nq, :], in1=gab[gi][0:nq, db * 512:(db + 1) * 512],
                            op=ALU.mult), reads=[yk, ("gab", gi)], writes=[("x1", 0, db)])
                        A("pool", lambda e, db=db, i=i, nq=nq: e.tensor_tensor(
                            out=x1[0][0:nq, db * 512:(db + 1) * 512], in0=x1[0][0:nq, db * 512:(db + 1) * 512],
                            in1=xt[0][0:nq, db * 512:(db + 1) * 512], op=ALU.add),
                          reads=[("x1", 0, db), ("xt", 0)], writes=[("x1", 0, db)])
                    x1k = [("x1", 0, db) for db in range(4)]
                    A("sp", lambda e, i=i, nq=nq, tok0=tok0: e.dma_start(out=x1S[tok0:tok0 + nq, :], in_=x1[0][0:nq, :]),
                      reads=x1k, writes=["x1S"], dma=True)
                    A("act", lambda e, i=i, nq=nq: e.activation(out=junk[0:nq, :], in_=x1[0][0:nq, :], func=AF.Square,
                                                                accum_out=ss[i][0:nq, 0:1]), reads=x1k, writes=["junk4", ("ss4", i)])
                    A("dve", lambda e, i=i, nq=nq: e.tensor_scalar(out=ss[i][0:nq, 1:2], in0=ss[i][0:nq, 0:1], scalar1=1.0 / D,
                                                                   scalar2=EPS, op0=ALU.mult, op1=ALU.add),
                      reads=[("ss4", i)], writes=[("rs4", i)])
                    A("act", lambda e, i=i, nq=nq: e.activation(out=ss[i][0:nq, 1:2], in_=ss[i][0:nq, 1:2], func=AF.Sqrt),
                      reads=[("rs4", i)], writes=[("rs4", i)])
                    A("dve", lambda e, i=i, nq=nq: e.reciprocal(out=ss[i][0:nq, 1:2], in_=ss[i][0:nq, 1:2]),
                      reads=[("rs4", i)], writes=[("rs4", i)])
                    A("dve", lambda e, i=i, nq=nq: e.tensor_scalar(out=xn[i][0:nq, :], in0=x1[0][0:nq, :], scalar1=ss[i][0:nq, 1:2],
                                                                   scalar2=None, op0=ALU.mult),
                      reads=x1k + [("rs4", i)], writes=[("xn4", i)])
                    for half in range(2):
                        pi, pk = rpt.next()
                        for k8 in range(8):
                            kt = half * 8 + k8
                            A("pe", lambda e, pi=pi, k8=k8, kt=kt, i=i, nq=nq: e.transpose(
                                pth[pi][:, k8, 0:nq], xn[i][0:nq, kt * 128:(kt + 1) * 128], ident_b[0:nq, 0:nq]),
                              reads=[("xn4", i), "ident_b"], writes=[pk])
                        ti, tk = rtm.next()
                        for (lo, hi, r) in segs:
                            w = hi - lo
                            A("dve", lambda e, pi=pi, ti=ti, lo=lo, hi=hi, r=r, w=w, half=half: e.tensor_tensor(
                                out=tmpf[ti][:, :, lo:hi], in0=pth[pi][:, :, lo:hi],
                                in1=AfT[:, r, half * 8:half * 8 + 8].unsqueeze(2).to_broadcast([128, 8, w]), op=ALU.mult),
                              reads=[pk, "AfT"], writes=[tk])
                            A("pool", lambda e, ti=ti, lo=lo, hi=hi, r=r, w=w, half=half, off=off: e.tensor_tensor(
                                out=h2T[:, half * 8:half * 8 + 8, off + lo:off + hi], in0=tmpf[ti][:, :, lo:hi],
                                in1=BfT[:, r, half * 8:half * 8 + 8].unsqueeze(2).to_broadcast([128, 8, w]), op=ALU.add),
                              reads=[tk, "BfT"], writes=[("h2T", tl)])
                    for kt in range(16):
                        A("pe", lambda e, kt=kt, off=off, nq=nq: e.matmul(pr[0:nq, :], lhsT=h2T[:, kt, off:off + nq], rhs=wr[:, kt, :],
                                                                      start=(kt == 0), stop=(kt == 15)),
                          reads=[("h2T", tl), "wr"], writes=["pr"])
                    R_ = rt[i]
                    Q_ = rs_[i]
                    rk = ("rt", i)
                    v3 = lambda ap: ap.rearrange("p (g k) -> p g k", g=8)
                    A("act", lambda e, R_=R_, nq=nq: e.activation(out=R_[0:nq, 0, :], in_=pr[0:nq, :], func=AF.Sigmoid),
                      reads=["pr"], writes=[rk])
                    A("dve", lambda e, R_=R_, nq=nq: e.tensor_tensor(out=R_[0:nq, 1, :], in0=R_[0:nq, 0, :], in1=rbias[0:nq, :],
                                                                     op=ALU.add), reads=[rk, "rbias"], writes=[rk])
                    A("dve", lambda e, R_=R_, Q_=Q_, nq=nq: e.tensor_reduce(out=Q_[0:nq, 0:8], in_=v3(R_[0:nq, 1, :]), axis=AX.X,
                                                                            op=ALU.max), reads=[rk], writes=[rk])
                    A("dve", lambda e, R_=R_, Q_=Q_, nq=nq: e.tensor_tensor(
                        out=v3(R_[0:nq, 2, :]), in0=v3(R_[0:nq, 1, :]), in1=Q_[0:nq, 0:8].unsqueeze(2).to_broadcast([nq, 8, 8]),
                        op=ALU.is_equal), reads=[rk], writes=[rk])
                    A("dve", lambda e, R_=R_, nq=nq: e.scalar_tensor_tensor(
                        out=R_[0:nq, 2, :], in0=R_[0:nq, 2, :], scalar=-BIG, in1=R_[0:nq, 1, :], op0=ALU.mult, op1=ALU.add),
                      reads=[rk], writes=[rk])
                    A("dve", lambda e, R_=R_, Q_=Q_, nq=nq: e.tensor_reduce(out=Q_[0:nq, 8:16], in_=v3(R_[0:nq, 2, :]), axis=AX.X,
                                                                            op=ALU.max), reads=[rk], writes=[rk])
                    A("dve", lambda e, Q_=Q_, nq=nq: e.tensor_tensor(out=Q_[0:nq, 16:24], in0=Q_[0:nq, 0:8], in1=Q_[0:nq, 8:16],
                                                                     op=ALU.add), reads=[rk], writes=[rk])
                    A("dve", lambda e, Q_=Q_, nq=nq: e.max(out=Q_[0:nq, 24:32], in_=Q_[0:nq, 16:24]), reads=[rk], writes=[rk])
                    A("dve", lambda e, Q_=Q_, nq=nq: e.tensor_scalar(out=Q_[0:nq, 32:40], in0=Q_[0:nq, 16:24], scalar1=Q_[0:nq, 27:28],
                                                                     scalar2=None, op0=ALU.is_ge), reads=[rk], writes=[rk])
                    A("dve", lambda e, Q_=Q_, nq=nq: e.tensor_scalar(out=Q_[0:nq, 40:48], in0=Q_[0:nq, 32:40], scalar1=BIG, scalar2=-BIG,
                                                                     op0=ALU.mult, op1=ALU.add), reads=[rk], writes=[rk])
                    A("dve", lambda e, R_=R_, Q_=Q_, nq=nq: e.tensor_tensor(
                        out=v3(R_[0:nq, 2, :]), in0=v3(R_[0:nq, 1, :]), in1=Q_[0:nq, 32:40].unsqueeze(2).to_broadcast([nq, 8, 8]),
                        op=ALU.mult), reads=[rk], writes=[rk])
                    A("dve", lambda e, R_=R_, Q_=Q_, nq=nq: e.tensor_tensor(
                        out=v3(R_[0:nq, 2, :]), in0=v3(R_[0:nq, 2, :]), in1=Q_[0:nq, 40:48].unsqueeze(2).to_broadcast([nq, 8, 8]),
                        op=ALU.add), reads=[rk], writes=[rk])
                    A("dve", lambda e, R_=R_, Q_=Q_, nq=nq: e.max(out=Q_[0:nq, 0:8], in_=R_[0:nq, 2, :]), reads=[rk], writes=[rk])
                    A("dve", lambda e, R_=R_, Q_=Q_, nq=nq: e.tensor_scalar(out=R_[0:nq, 3, :], in0=R_[0:nq, 2, :],
                                                                            scalar1=Q_[0:nq, 7:8], scalar2=None, op0=ALU.is_ge),
                      reads=[rk], writes=[rk])
                    A("dve", lambda e, R_=R_, nq=nq: e.tensor_tensor(out=R_[0:nq, 3, :], in0=R_[0:nq, 3, :], in1=R_[0:nq, 0, :],
                                                                     op=ALU.mult), reads=[rk], writes=[rk])
                    A("dve", lambda e, R_=R_, Q_=Q_, nq=nq: e.reduce_sum(out=Q_[0:nq, 8:9], in_=R_[0:nq, 3, :], axis=AX.X),
                      reads=[rk], writes=[rk])
                    A("dve", lambda e, Q_=Q_, nq=nq: e.reciprocal(out=Q_[0:nq, 9:10], in_=Q_[0:nq, 8:9]), reads=[rk], writes=[rk])
                    A("dve", lambda e, R_=R_, Q_=Q_, nq=nq: e.tensor_scalar(out=R_[0:nq, 4, :], in0=R_[0:nq, 3, :],
                                                                            scalar1=Q_[0:nq, 9:10], scalar2=2.5, op0=ALU.mult,
                                                                            op1=ALU.mult), reads=[rk], writes=[rk])
                    A("pe", lambda e, R_=R_, nq=nq: e.transpose(pg[0:NEXP, 0:nq], R_[0:nq, 4, :], ident_f[0:nq, 0:nq]),
                      reads=[rk, "ident_f"], writes=["pg"])
                    A("act", lambda e, nq=nq, off=off: e.activation(out=gatesT[0:NEXP, off:off + nq], in_=pg[0:NEXP, 0:nq], func=AF.Copy),
                      reads=["pg"], writes=["gatesT"])
                S.emit()
            with contextlib.ExitStack() as yes:
                Yacc = yes.enter_context(nc.sbuf_tensor("Yacc_h%d" % hf, [128, 9, D], F32))
                with contextlib.ExitStack() as es:
                    def sb(name, shape, dt):
                        return es.enter_context(nc.sbuf_tensor(name + "_h%d" % hf, list(shape), dt))

                    def ps(name, shape, dt=F32):
                        return es.enter_context(nc.psum_tensor(name + "_h%d" % hf, list(shape), dt))

                    wsl = [sb("wsl%d" % i, [128, 8192], BF16) for i in range(3)]
                    GU = [sb("GU%d" % i, [128, 4, 512], BF16) for i in range(2)]
                    sgt = [sb("sgt%d" % i, [128, 512], F32) for i in range(2)]
                    t1 = [sb("t1_%d" % i, [128, 512], F32) for i in range(2)]
                    gbs = [sb("gbs%d" % i, [128, 512], F32) for i in range(2)]
                    pG = [ps("pG%d" % i, [128, 512]) for i in range(2)]
                    pU = [ps("pU%d" % i, [128, 512]) for i in range(2)]
                    pgb = ps("pgb", [128, 512])
                    pY = [ps("pY%d" % i, [128, 512]) for i in range(3)]
                    rws, rGU, rsg, rt1, rgb, rpG, rpY = (Rot("wsl", 3), Rot("GU", 2), Rot("sgt", 2), Rot("t1", 2), Rot("gbs", 2),
                                                         Rot("pGU", 2), Rot("pY", 3))
                    hkeys = [("h2T", t[0]) for t in tiles]
                    for ex in range(NEXP + 1):
                        if ex < NEXP:
                            srcs = (w_gate[ex], w_up[ex], w_down[ex])
                        else:
                            srcs = (ws_gate, ws_up, ws_down)
                        wi = []
                        for k_, src in enumerate(srcs):
                            si, sk = rws.next()
                            wq = ("wq", (3 * ex + k_) % 2)
                            if k_ < 2:
                                view = wsl[si][:].rearrange("p (kt f) -> p kt f", kt=16)
                                A("pool", lambda e, view=view, src=src: e.dma_start(
                                    out=view, in_=src.rearrange("(kt p) f -> p kt f", p=128)), writes=[sk, wq], dma=True)
                            else:
                                view = wsl[si][:].rearrange("p (ft d) -> p ft d", ft=4)
                                A("pool", lambda e, view=view, src=src: e.dma_start(
                                    out=view, in_=src.rearrange("(ft p) d -> p ft d", p=128)), writes=[sk, wq], dma=True)
                            wi.append((view, sk))
                        (wg, wgk), (wu, wuk), (wd, wdk) = wi
                        for (b0, nb) in blocks:
                            A("pe", lambda e, ex=ex, b0=b0, nb=nb: e.matmul(
                                pgb[:, 0:nb], lhsT=ident_f[0:65, ex:ex + 1].to_broadcast([65, 128]), rhs=gatesT[0:65, b0:b0 + nb],
                                start=True, stop=True), reads=["gatesT", "ident_f"], writes=["pgb"])
                            gbi, gbk = rgb.next()
                            A("act", lambda e, gbi=gbi, nb=nb: e.activation(out=gbs[gbi][:, 0:nb], in_=pgb[:, 0:nb], func=AF.Copy),
                              reads=["pgb"], writes=[gbk])
                            gi, gk = rGU.next()
                            for ft in range(4):
                                pi, pk = rpG.next()
                                for kt in range(16):
                                    A("pe", lambda e, pi=pi, kt=kt, ft=ft, b0=b0, nb=nb, wg=wg: e.matmul(
                                        pG[pi][:, 0:nb], lhsT=wg[:, kt, ft * 128:(ft + 1) * 128], rhs=h2T[:, kt, b0:b0 + nb],
                                        start=(kt == 0), stop=(kt == 15)), reads=[wgk] + hkeys, writes=[("pG", pi)])
                                for kt in range(16):
                                    A("pe", lambda e, pi=pi, kt=kt, ft=ft, b0=b0, nb=nb, wu=wu: e.matmul(
                                        pU[pi][:, 0:nb], lhsT=wu[:, kt, ft * 128:(ft + 1) * 128], rhs=h2T[:, kt, b0:b0 + nb],
                                        start=(kt == 0), stop=(kt == 15)), reads=[wuk] + hkeys, writes=[("pU", pi)])
                                sgi, sgk = rsg.next()
                                A("act", lambda e, pi=pi, sgi=sgi, nb=nb: e.activation(out=sgt[sgi][:, 0:nb], in_=pG[pi][:, 0:nb],
                                                                                       func=AF.Silu), reads=[("pG", pi)], writes=[sgk])
                                ti, tk = rt1.next()
                                A("dve", lambda e, pi=pi, sgi=sgi, ti=ti, nb=nb: e.tensor_tensor(
                                    out=t1[ti][:, 0:nb], in0=pU[pi][:, 0:nb], in1=sgt[sgi][:, 0:nb], op=ALU.mult),
                                  reads=[("pU", pi), sgk], writes=[tk])
                                A("pool", lambda e, ti=ti, gi=gi, ft=ft, gbi=gbi, nb=nb: e.tensor_tensor(
                                    out=GU[gi][:, ft, 0:nb], in0=t1[ti][:, 0:nb], in1=gbs[gbi][:, 0:nb], op=ALU.mult),
                                  reads=[tk, gbk], writes=[gk])
                            for (tl, nq, off, tok0, xsrc, segs) in tiles:
                                if not (b0 <= off < b0 + nb):
                                    continue
                                lo = off - b0
                                for db in range(4):
                                    yi, yk = rpY.next()
                                    for ft in range(4):
                                        A("pe", lambda e, yi=yi, ft=ft, gi=gi, lo=lo, nq=nq, db=db, wd=wd: e.matmul(
                                            pY[yi][0:nq, :], lhsT=GU[gi][:, ft, lo:lo + nq], rhs=wd[:, ft, db * 512:(db + 1) * 512],
                                            start=(ft == 0), stop=(ft == 3)), reads=[gk, wdk], writes=[yk])
                                    if ex == 0:
                                        A("dve", lambda e, yi=yi, tl=tl, db=db, nq=nq: e.tensor_copy(
                                            Yacc[0:nq, tl, db * 512:(db + 1) * 512], pY[yi][0:nq, :]), reads=[yk], writes=[("Y", tl, db)])
                                    else:
                                        A("dve", lambda e, yi=yi, tl=tl, db=db, nq=nq: e.tensor_tensor(
                                            out=Yacc[0:nq, tl, db * 512:(db + 1) * 512], in0=pY[yi][0:nq, :],
                                            in1=Yacc[0:nq, tl, db * 512:(db + 1) * 512], op=ALU.add),
                                          reads=[yk, ("Y", tl, db)], writes=[("Y", tl, db)])
                    S.emit()
                with contextlib.ExitStack() as es:
                    def sb(name, shape, dt):
                        return es.enter_context(nc.sbuf_tensor(name + "_h%d" % hf, list(shape), dt))

                    gfb = [sb("gfb%d" % i, [128, D], F32) for i in range(2)]
                    fgb = sb("fgb", [128, D], F32)
                    x1t = [sb("x1t%d" % i, [128, D], F32) for i in range(2)]
                    yt = [sb("yt%d" % i, [128, D], F32) for i in range(2)]
                    junk = sb("junk5", [128, D], BF16)
                    ss = [sb("ss5%d" % i, [128, 2], F32) for i in range(2)]
                    A("sp", lambda e: e.dma_start(out=gfb[0][:], in_=modS[0:1, 10240:12288].partition_broadcast(128)),
                      writes=[("gfb", 0)], dma=True)
                    if hf == 1:
                        for r in range(2):
                            A("sp", lambda e, r=r: e.dma_start(out=gfb[1][16 * r:16 * r + 16, :],
                                                               in_=modS[1 + r:2 + r, 10240:12288].partition_broadcast(16)),
                              writes=[("gfb", 1)], dma=True)
                    A("sp", lambda e: e.dma_start(out=fgb[:], in_=final_g.partition_broadcast(128)), writes=["fgb"], dma=True)
                    for (tl, nq, off, tok0, xsrc, segs) in tiles:
                        i = tl % 2
                        gi = 1 if nq == 32 else 0
                        A("sp", lambda e, i=i, nq=nq, tok0=tok0: e.dma_start(out=x1t[i][0:nq, :], in_=x1S[tok0:tok0 + nq, :]),
                          writes=[("x1t", i)], dma=True)
                        A("dve", lambda e, i=i, nq=nq, tl=tl, gi=gi: e.tensor_tensor(
                            out=yt[i][0:nq, :], in0=Yacc[0:nq, tl, :], in1=gfb[gi][0:nq, :], op=ALU.mult),
                          reads=[("gfb", gi)], writes=[("yt", i)])
                        A("pool", lambda e, i=i, nq=nq: e.tensor_tensor(out=yt[i][0:nq, :], in0=yt[i][0:nq, :], in1=x1t[i][0:nq, :],
                                                                        op=ALU.add), reads=[("yt", i), ("x1t", i)], writes=[("yt", i)])
                        A("act", lambda e, i=i, nq=nq: e.activation(out=junk[0:nq, :], in_=yt[i][0:nq, :], func=AF.Square,
                                                                    accum_out=ss[i][0:nq, 0:1]), reads=[("yt", i)], writes=["junk5", ("ss5", i)])
                        A("dve", lambda e, i=i, nq=nq: e.tensor_scalar(out=ss[i][0:nq, 1:2], in0=ss[i][0:nq, 0:1], scalar1=1.0 / D,
                                                                       scalar2=EPS, op0=ALU.mult, op1=ALU.add),
                          reads=[("ss5", i)], writes=[("rs5", i)])
                        A("act", lambda e, i=i, nq=nq: e.activation(out=ss[i][0:nq, 1:2], in_=ss[i][0:nq, 1:2], func=AF.Sqrt),
                          reads=[("rs5", i)], writes=[("rs5", i)])
                        A("dve", lambda e, i=i, nq=nq: e.reciprocal(out=ss[i][0:nq, 1:2], in_=ss[i][0:nq, 1:2]),
                          reads=[("rs5", i)], writes=[("rs5", i)])
                        A("dve", lambda e, i=i, nq=nq: e.scalar_tensor_tensor(
                            out=yt[i][0:nq, :], in0=yt[i][0:nq, :], scalar=ss[i][0:nq, 1:2], in1=fgb[0:nq, :], op0=ALU.mult,
                            op1=ALU.mult), reads=[("yt", i), ("rs5", i), "fgb"], writes=[("yt", i)])
                        if nq == 32:
                            A("sp", lambda e, i=i: e.dma_start(out=y_smp, in_=yt[i][0:32, :]), reads=[("yt", i)], dma=True)
                        else:
                            A("sp", lambda e, i=i, tok0=tok0: e.dma_start(out=y_own[tok0:tok0 + 128, :], in_=yt[i][:, :]),
                              reads=[("yt", i)], dma=True)
                    S.emit()


def _bucket_np(rel):
    import jax
    import jax.numpy as jnp
    import math
    with jax.default_device(jax.devices("cpu")[0]):
        rel = jnp.asarray(rel, dtype=jnp.int32)
        nb, max_exact = 16, 8
        n = jnp.abs(rel)
        nf = jnp.maximum(n, 1).astype(jnp.float32)
        large = max_exact + (jnp.log(nf / max_exact) / math.log(128 / max_exact) * (nb - max_exact)).astype(jnp.int32)
        large = jnp.minimum(large, nb - 1)
        out = jnp.where(rel > 0, nb, 0) + jnp.where(n < max_exact, n, large)
        return np.asarray(out)


def _consts(j):
    s = np.arange(128)[:, None]
    q = np.arange(128)[None, :]
    bd = _bucket_np(s - q)
    bp = _bucket_np(s - 128 - q)
    visd = ((s // 64) <= (q // 64))
    cb = np.zeros((7, 32, 128, 128), np.float32)
    neg = np.zeros((7, 128, 128), np.float32)

    def fill(u, bk, vis):
        for b in range(32):
            m = (bk == b).astype(np.float32) - (1.0 if b == 15 else 0.0)
            cb[u, b] = m.T
        if vis is not None:
            neg[u] = np.where(vis, 0.0, NEGV)
    for v in range(5):
        delta = v - 1 - j
        if delta == 0:
            fill(v, bd, visd)
        elif delta == -1:
            fill(v, bp, None)
        elif delta > 0:
            neg[v] = NEGV
    fill(5, bp, None)
    fill(6, bd, visd)
    v01 = np.zeros((128, 4, 128), np.float32)
    vng = np.zeros((128, 4, 128), np.float32)
    for u in range(4):
        d = u - j
        if d < 0:
            v01[:, u, :] = 1.0
        elif d == 0:
            v01[:, u, :] = visd.T.astype(np.float32)
            vng[:, u, :] = np.where(visd.T, 0.0, -1e30)
        else:
            vng[:, u, :] = -1e30
    return cb.reshape(7, 32, 128 * 128), neg, v01.reshape(128, 512), vng.reshape(128, 512)


_CACHE = {}


def kernel(**inp):
    f = lambda a: np.ascontiguousarray(np.asarray(a, dtype=np.float32))
    if "nc" not in _CACHE:
        _CACHE["nc"] = build_program()
    nc = _CACHE["nc"]
    xp = f(inp["x_prompt"])
    xs = f(inp["x_sample"])
    shared = dict(
        relb=f(inp["rel_bias"]).reshape(1, 384), w_ada=f(inp["w_ada"])[0], b_ada=f(inp["b_ada"]).reshape(1, 6 * D),
        norm_a_g=f(inp["norm_a_g"]).reshape(1, D), w_in=f(inp["w_in"])[0], w_out=f(inp["w_out"])[0],
        diff_lam=f(inp["diff_lam"]).reshape(1, 512), subln_g=f(inp["subln_g"]).reshape(1, 256),
        norm_f_g=f(inp["norm_f_g"]).reshape(1, D), w_router=f(inp["w_router"])[0],
        router_bias=f(inp["router_bias"]).reshape(1, NEXP), w_gate=f(inp["w_gate"])[0], w_up=f(inp["w_up"])[0],
        w_down=f(inp["w_down"])[0], ws_gate=f(inp["ws_gate"])[0], ws_up=f(inp["ws_up"])[0], ws_down=f(inp["ws_down"])[0],
        final_g=f(inp["final_g"]).reshape(1, D))
    cp, cs = f(inp["c_prompt"]), f(inp["c_sample"])
    cak, cav, cki = f(inp["cache_a_k"])[0], f(inp["cache_a_v"])[0], f(inp["cache_a_kidx"])[0]
    cbk, cbv = f(inp["cache_b_k"])[0], f(inp["cache_b_v"])[0]
    in_maps = []
    for c in range(8):
        b, j = c // 4, c % 4
        cbc, neg, v01, vng = _consts(j)
        m = dict(shared)
        m.update(
            xb=xp[b], xown=np.ascontiguousarray(xp[b].reshape(64, 128, D)[j::4].reshape(2048, D)),
            xsmp=np.ascontiguousarray(xs[2 * c:2 * c + 2].reshape(32, D)),
            c3=np.ascontiguousarray(np.stack([cp[b], cs[2 * c], cs[2 * c + 1]])),
            cak=np.ascontiguousarray(cak[2 * c:2 * c + 2].reshape(2, PAST, 256)),
            cav=np.ascontiguousarray(cav[2 * c:2 * c + 2].reshape(2, PAST, 256)),
            cki=np.ascontiguousarray(cki[2 * c:2 * c + 2].reshape(2, PAST, 64)),
            cbk=np.ascontiguousarray(cbk[2 * c:2 * c + 2].reshape(2, PAST, 1024)),
            cbv=np.ascontiguousarray(cbv[2 * c:2 * c + 2].reshape(2, PAST, 1024)),
            cbc=cbc, negc=neg, vis01=v01, visneg=vng)
        in_maps.append(m)
    if os.environ.get("MK_CORES"):
        cs_ = [int(v) for v in os.environ["MK_CORES"].split(",")]
        res = run_bass_kernel_spmd(nc, [in_maps[c] for c in cs_], core_ids=list(range(len(cs_))))
        _CACHE["R"] = {c: r for c, r in zip(cs_, res.results)}
        return None
    res = run_bass_kernel_spmd(nc, in_maps, core_ids=list(range(8)))
    R = res.results
    y_p = np.zeros((2, SEQ, D), np.float32)
    y_s = np.zeros((16, 16, D), np.float32)
    rows_p = {k: np.zeros((2, SEQ, w), np.float32) for k, w in (("ak", 256), ("av", 256), ("ki", 64), ("bk", 1024), ("bv", 1024))}
    rows_s = {k: np.zeros((16, 16, w), np.float32) for k, w in (("ak", 256), ("av", 256), ("ki", 64), ("bk", 1024), ("bv", 1024))}
    for c in range(8):
        b, j = c // 4, c % 4
        r = R[c]
        y_p[b].reshape(64, 128, D)[j::4] = np.asarray(r["y_own"]).reshape(16, 128, D)
        y_s[2 * c:2 * c + 2] = np.asarray(r["y_smp"]).reshape(2, 16, D)
        for k, w in (("ak", 256), ("av", 256), ("ki", 64), ("bk", 1024), ("bv", 1024)):
            rows_p[k][b].reshape(64, 128, w)[j::4] = np.asarray(r[k + "_own"]).reshape(16, 128, w)
            rows_s[k][2 * c:2 * c + 2] = np.asarray(r[k + "_s"]).reshape(2, 16, w)
    return (y_p, y_s,
            rows_p["ak"].reshape(1, 2, SEQ, 2, 128), rows_p["av"].reshape(1, 2, SEQ, 2, 128), rows_p["ki"].reshape(1, 2, SEQ, 64),
            rows_p["bk"].reshape(1, 2, SEQ, 4, 2, 128), rows_p["bv"].reshape(1, 2, SEQ, 4, 256),
            rows_s["ak"].reshape(1, 16, 16, 2, 128), rows_s["av"].reshape(1, 16, 16, 2, 128), rows_s["ki"].reshape(1, 16, 16, 64),
            rows_s["bk"].reshape(1, 16, 16, 4, 2, 128), rows_s["bv"].reshape(1, 16, 16, 4, 256))
```

```python
import os
import contextlib
import numpy as np
import concourse.bass as bass
import concourse.mybir as mybir
from concourse.bass_utils import run_bass_kernel_spmd

F32 = mybir.dt.float32
BF16 = mybir.dt.bfloat16
ALU = mybir.AluOpType
AF = mybir.ActivationFunctionType
AX = mybir.AxisListType

D = 2048
DIN = 5712
SEQ = 8192
PAST = 4096
LS = PAST + 16
LSP = 4224
NEXP = 64
EPS = 1e-6
NEGV = -30000.0
STAGE = int(os.environ.get("MK_STAGE", "9"))
NBIS = 20

ENGS = ("pe", "act", "dve", "pool", "sp")


class Op:
    __slots__ = ("eng", "fn", "deps", "is_dma", "sig", "has_dep", "dsem", "dval")

    def __init__(self, eng, fn, is_dma):
        self.eng = eng
        self.fn = fn
        self.is_dma = is_dma
        self.deps = []
        self.sig = None
        self.has_dep = False


class Sched:
    def __init__(self, nc, es, ndma=8):
        self.nc = nc
        self.ndma = ndma
        self.csem = {e: es.enter_context(nc.semaphore("c_" + e)) for e in ENGS}
        self.dsem = {e: [es.enter_context(nc.semaphore("d_%s%d" % (e, i))) for i in range(ndma)]
                     for e in ("sp", "pool")}
        self.cnt = {e: 0 for e in ENGS}
        self.dcnt = {e: 0 for e in self.dsem}
        self.first = True
        self.reset()

    def reset(self):
        self.ops = {e: [] for e in ENGS}
        self.state = {}

    def add(self, eng, fn, reads=(), writes=(), dma=False):
        op = Op(eng, fn, dma)
        deps = []
        for k in reads:
            st = self.state.get(k)
            if st is None:
                st = self.state[k] = [None, []]
            if st[0] is not None:
                deps.append(st[0])
            st[1].append(op)
        for k in writes:
            st = self.state.get(k)
            if st is None:
                st = self.state[k] = [None, []]
            if st[0] is not None:
                deps.append(st[0])
            for r in st[1]:
                if r is not op:
                    deps.append(r)
            st[0] = op
            st[1] = []
        seen = set()
        for d in deps:
            if id(d) in seen or d is op:
                continue
            if eng == "pe" and d.eng == "pe" and not d.is_dma:
                continue
            seen.add(id(d))
            op.deps.append(d)
            d.has_dep = True
        self.ops[eng].append(op)
        return op

    def emit(self):
        nc = self.nc
        start_c = dict(self.cnt)
        start_d = {}
        for e in self.dsem:
            for i in range(self.ndma):
                n_i = (self.dcnt[e] - i + self.ndma - 1) // self.ndma if self.dcnt[e] > i else 0
                start_d[(e, i)] = 16 * n_i
        first = self.first
        self.first = False
        for e in ENGS:
            last = None
            for op in self.ops[e]:
                if not op.is_dma and op.fn is not None:
                    last = op
            if last is not None:
                last.has_dep = True
            for op in self.ops[e]:
                if op.is_dma:
                    n = self.dcnt[e]
                    op.dsem = self.dsem[e][n % self.ndma]
                    op.dval = 16 * (n // self.ndma + 1)
                    self.dcnt[e] = n + 1
                elif op.has_dep:
                    self.cnt[e] += 1
                    op.sig = self.cnt[e]
        ops = self.ops
        csem, dsem, ndma = self.csem, self.dsem, self.ndma

        def run(ename, engobj):
            waited = {}

            def wait(sem, val):
                key = id(sem)
                if val <= 0 or waited.get(key, 0) >= val:
                    return
                waited[key] = val
                engobj.wait_ge(sem, val)

            if not ops[ename]:
                return
            if not first:
                for f in ENGS:
                    wait(csem[f], start_c[f])
                for (q, i), v in start_d.items():
                    wait(dsem[q][i], v)
            for op in ops[ename]:
                for d in op.deps:
                    if d.is_dma:
                        wait(d.dsem, d.dval)
                    else:
                        wait(csem[d.eng], d.sig)
                if op.is_dma:
                    wait(op.dsem, op.dval - 16)
                    ins = op.fn(engobj)
                    ins.then_inc(op.dsem, 16)
                else:
                    ins = op.fn(engobj)
                    if op.sig is not None:
                        ins.then_inc(csem[ename], 1)
            if ename in dsem:
                lastd = {}
                for op in ops[ename]:
                    if op.is_dma:
                        lastd[id(op.dsem)] = (op.dsem, op.dval)
                for s, v in lastd.values():
                    wait(s, v)

        with contextlib.ExitStack() as _scs:
            _scs.enter_context(nc.allow_non_contiguous_dma(reason="small strided parameter loads"))
            if os.environ.get("MK_SCOPES"):
                self.nscope = getattr(self, "nscope", 0) + 1
                _scs.enter_context(nc.named_scope("emit%02d" % self.nscope))
            with nc.Block() as block:
                @block.tensor
                def _(e):
                    run("pe", e)

                @block.scalar
                def _(e):
                    run("act", e)

                @block.vector
                def _(e):
                    run("dve", e)

                @block.gpsimd
                def _(e):
                    run("pool", e)

                @block.sync
                def _(e):
                    run("sp", e)
        self.reset()


class Rot:
    def __init__(self, name, n):
        self.name, self.n, self.i = name, n, 0

    def next(self):
        i = self.i % self.n
        self.i += 1
        return i, (self.name, i)


def build_program():
    nc = bass.Bass("TRN2", target_bir_lowering=False)

    def din(name, shape, dt=F32):
        return nc.dram_tensor(name, list(shape), dt, kind="ExternalInput").ap()

    def dout(name, shape):
        return nc.dram_tensor(name, list(shape), F32, kind="ExternalOutput").ap()

    def dscr(name, shape, dt):
        return nc.dram_tensor(name, list(shape), dt, kind="Internal").ap()

    xb = din("xb", [SEQ, D])
    xown = din("xown", [2048, D])
    xsmp = din("xsmp", [32, D])
    c3 = din("c3", [3, D])
    cak = din("cak", [2, PAST, 256])
    cav = din("cav", [2, PAST, 256])
    cki = din("cki", [2, PAST, 64])
    cbk = din("cbk", [2, PAST, 1024])
    cbv = din("cbv", [2, PAST, 1024])
    relb = din("relb", [1, 384])
    w_ada = din("w_ada", [D, 6 * D])
    b_ada = din("b_ada", [1, 6 * D])
    norm_a_g = din("norm_a_g", [1, D])
    w_in = din("w_in", [D, DIN])
    w_out = din("w_out", [D, D])
    diff_lam = din("diff_lam", [1, 512])
    subln_g = din("subln_g", [1, 256])
    norm_f_g = din("norm_f_g", [1, D])
    w_router = din("w_router", [D, NEXP])
    router_bias = din("router_bias", [1, NEXP])
    w_gate = din("w_gate", [NEXP, D, 512])
    w_up = din("w_up", [NEXP, D, 512])
    w_down = din("w_down", [NEXP, 512, D])
    ws_gate = din("ws_gate", [D, 512])
    ws_up = din("ws_up", [D, 512])
    ws_down = din("ws_down", [512, D])
    final_g = din("final_g", [1, D])
    cbc = din("cbc", [7, 32, 128 * 128])
    negc = din("negc", [7, 128, 128])
    vis01 = din("vis01", [128, 512])
    visneg = din("visneg", [128, 512])

    y_own = dout("y_own", [2048, D])
    y_smp = dout("y_smp", [32, D])
    ak_own = dout("ak_own", [2048, 256])
    av_own = dout("av_own", [2048, 256])
    ki_own = dout("ki_own", [2048, 64])
    bk_own = dout("bk_own", [2048, 1024])
    bv_own = dout("bv_own", [2048, 1024])
    ak_s = dout("ak_s", [32, 256])
    av_s = dout("av_s", [32, 256])
    ki_s = dout("ki_s", [32, 64])
    bk_s = dout("bk_s", [32, 1024])
    bv_s = dout("bv_s", [32, 1024])

    modS = dscr("modS", [3, 6 * D], F32)
    Ls = [SEQ, LSP, LSP]
    KAT = [dscr("KAT%d" % i, [2, 128, Ls[i]], BF16) for i in range(3)]
    VA = [dscr("VA%d" % i, [Ls[i], 256], BF16) for i in range(3)]
    KIT = [dscr("KIT%d" % i, [64, Ls[i]], BF16) for i in range(3)]
    KBT = [dscr("KBT%d" % i, [8, 128, Ls[i]], BF16) for i in range(3)]
    VB = [dscr("VB%d" % i, [Ls[i], 1024], BF16) for i in range(3)]
    QAT = dscr("QAT", [8, 128, 2080], BF16)
    QBT = dscr("QBT", [8, 128, 2080], BF16)
    QIT = dscr("QIT", [8, 128, 2080], BF16)
    IWS = dscr("IWS", [2080, 16], F32)
    if os.environ.get("MK_DEBUG"):
        mixS = nc.dram_tensor("mixS", [2080, D], BF16, kind="ExternalOutput").ap()
    else:
        mixS = dscr("mixS", [2080, D], BF16)
    if os.environ.get("MK_DEBUG"):
        x1S = nc.dram_tensor("x1S", [2080, D], F32, kind="ExternalOutput").ap()
    else:
        x1S = dscr("x1S", [2080, D], F32)

    with contextlib.ExitStack() as ges:
        S = Sched(nc, ges)

        def gsb(name, shape, dt):
            return ges.enter_context(nc.sbuf_tensor(name, list(shape), dt))

        ident_b = gsb("ident_b", [128, 128], BF16)
        ident_f = gsb("ident_f", [128, 128], F32)
        ones_b = gsb("ones_b", [128, 1], BF16)
        onesrow = gsb("onesrow", [1, 128], F32)
        AaT = gsb("AaT", [128, 3, 16], F32)
        BaT = gsb("BaT", [128, 3, 16], F32)
        AfT = gsb("AfT", [128, 3, 16], F32)
        BfT = gsb("BfT", [128, 3, 16], F32)
        lamt = gsb("lamt", [128, 4], F32)
        sg8 = gsb("sg8", [128, 256], F32)
        rbias = gsb("rbias", [128, NEXP], F32)
        mbes = contextlib.ExitStack()
        MB = mbes.enter_context(nc.sbuf_tensor("MB", [128, 7, 12, 128], BF16))

        with contextlib.ExitStack() as es:
            def sb(name, shape, dt):
                return es.enter_context(nc.sbuf_tensor(name, list(shape), dt))

            def ps(name, shape, dt=F32):
                return es.enter_context(nc.psum_tensor(name, list(shape), dt))

            scT = sb("scT", [128, 16, 3], F32)
            wblk = [sb("wblk%d" % i, [128, 16, 512], F32) for i in range(2)]
            bblk = [sb("bblk%d" % i, [1, 512], F32) for i in range(2)]
            mblk = [sb("mblk%d" % i, [3, 512], F32) for i in range(2)]
            pm = [ps("pm%d" % i, [128, 512]) for i in range(2)]
            pmb = ps("pmb", [128, 128, 16])
            cbt = sb("cbt", [32, 128 * 128], F32)
            negt = sb("negt", [128, 7, 128], F32)
            tab = sb("tab", [32, 12], F32)
            t16 = [sb("t16_%d" % i, [128, 3, 16], F32) for i in range(4)]
            g16 = [sb("g16_%d" % i, [128, 16], F32) for i in range(2)]
            dl = sb("dl", [128, 512], F32)
            dtmp = sb("dtmp", [128, 256], F32)
            sgl = sb("sgl", [128, 256], F32)

            A = S.add
            A("pool", lambda e: e.memset(ident_b[:], 1.0), writes=["ident_b"])
            A("pool", lambda e: e.affine_select(out=ident_b[:], in_=ident_b[:], pattern=[[-1, 128]],
                                                compare_op=ALU.is_equal, fill=0.0, base=0, channel_multiplier=1),
              reads=["ident_b"], writes=["ident_b"])
            A("pool", lambda e: e.memset(ident_f[:], 1.0), writes=["ident_f"])
            A("pool", lambda e: e.affine_select(out=ident_f[:], in_=ident_f[:], pattern=[[-1, 128]],
                                                compare_op=ALU.is_equal, fill=0.0, base=0, channel_multiplier=1),
              reads=["ident_f"], writes=["ident_f"])
            A("pool", lambda e: e.memset(ones_b[:], 1.0), writes=["ones_b"])
            A("pool", lambda e: e.memset(onesrow[:], 1.0), writes=["onesrow"])
            for r in range(3):
                A("sp", lambda e, r=r: e.dma_start(out=scT[:, :, r], in_=c3[r].rearrange("(kt p) -> p kt", p=128)),
                  writes=["scT"], dma=True)
            A("act", lambda e: e.activation(out=scT[:], in_=scT[:], func=AF.Silu), reads=["scT"], writes=["scT"])
            wv = w_ada.rearrange("(kt p) n -> p kt n", p=128)
            for nb in range(24):
                i = nb % 2
                A("sp", lambda e, i=i, nb=nb: e.dma_start(out=wblk[i][:], in_=wv[:, :, nb * 512:(nb + 1) * 512]),
                  writes=[("wblk", i)], dma=True)
                A("sp", lambda e, i=i, nb=nb: e.dma_start(out=bblk[i][:], in_=b_ada[:, nb * 512:(nb + 1) * 512]),
                  writes=[("bblk", i)], dma=True)
                for kt in range(16):
                    A("pe", lambda e, i=i, kt=kt: e.matmul(pm[i][0:3, :], lhsT=scT[:, kt, :], rhs=wblk[i][:, kt, :],
                                                           start=(kt == 0), stop=False),
                      reads=["scT", ("wblk", i)], writes=[("pm", i)])
                A("pe", lambda e, i=i: e.matmul(pm[i][0:3, :], lhsT=onesrow[0:1, 0:3], rhs=bblk[i][:],
                                                start=False, stop=True),
                  reads=["onesrow", ("bblk", i)], writes=[("pm", i)])
                A("dve", lambda e, i=i: e.tensor_copy(mblk[i][:], pm[i][0:3, :]), reads=[("pm", i)], writes=[("mblk", i)])
                A("sp", lambda e, i=i, nb=nb: e.dma_start(out=modS[:, nb * 512:(nb + 1) * 512], in_=mblk[i][:]),
                  reads=[("mblk", i)], writes=["modS"], dma=True)
            for ti, off in enumerate((0, 2048, 6144, 8192)):
                for r in range(3):
                    if ti < 2:
                        srcv = modS[r, off:off + 2048].rearrange("(kt p) -> p kt", p=128)
                    else:
                        srcv = modS[r, off:off + 2048].rearrange("(p kt) -> p kt", kt=16)
                    A("sp", lambda e, ti=ti, r=r, srcv=srcv: e.dma_start(out=t16[ti][:, r, :], in_=srcv),
                      reads=["modS"], writes=[("t16", ti)], dma=True)
            A("sp", lambda e: e.dma_start(out=g16[0][:], in_=norm_a_g.rearrange("o (kt p) -> p (o kt)", p=128)),
              writes=[("g16", 0)], dma=True)
            A("sp", lambda e: e.dma_start(out=g16[1][:], in_=norm_f_g.rearrange("o (p kt) -> p (o kt)", kt=16)),
              writes=[("g16", 1)], dma=True)
            for (dst, dname, sci, shi, gi) in ((AaT, "AaT", 1, 0, 0), (AfT, "AfT", 3, 2, 1)):
                A("dve", lambda e, sci=sci: e.tensor_scalar(out=t16[sci][:], in0=t16[sci][:], scalar1=1.0, scalar2=None,
                                                            op0=ALU.add),
                  reads=[("t16", sci)], writes=[("t16", sci)])
                A("dve", lambda e, dst=dst, sci=sci, gi=gi: e.tensor_tensor(
                    out=dst[:], in0=t16[sci][:], in1=g16[gi][:].unsqueeze(1).to_broadcast([128, 3, 16]), op=ALU.mult),
                  reads=[("t16", sci), ("g16", gi)], writes=[dname])
            A("dve", lambda e: e.tensor_copy(BaT[:], t16[0][:]), reads=[("t16", 0)], writes=["BaT"])
            A("dve", lambda e: e.tensor_copy(BfT[:], t16[2][:]), reads=[("t16", 2)], writes=["BfT"])
            A("sp", lambda e: e.dma_start(out=dl[:], in_=diff_lam.partition_broadcast(128)), writes=["dl"], dma=True)
            A("dve", lambda e: e.tensor_tensor(out=dtmp[:, 0:128], in0=dl[:, 0:128], in1=dl[:, 128:256], op=ALU.mult),
              reads=["dl"], writes=["dtmp0"])
            A("dve", lambda e: e.tensor_tensor(out=dtmp[:, 128:256], in0=dl[:, 256:384], in1=dl[:, 384:512], op=ALU.mult),
              reads=["dl"], writes=["dtmp1"])
            A("dve", lambda e: e.reduce_sum(out=lamt[:, 2:3], in_=dtmp[:, 0:128], axis=AX.X), reads=["dtmp0"], writes=["lam2"])
            A("dve", lambda e: e.reduce_sum(out=lamt[:, 3:4], in_=dtmp[:, 128:256], axis=AX.X), reads=["dtmp1"], writes=["lam3"])
            A("act", lambda e: e.activation(out=lamt[:, 2:4], in_=lamt[:, 2:4], func=AF.Exp), reads=["lam2", "lam3"],
              writes=["lam23"])
            A("dve", lambda e: e.tensor_tensor(out=lamt[:, 0:1], in0=lamt[:, 2:3], in1=lamt[:, 3:4], op=ALU.subtract),
              reads=["lam23"], writes=["lam0"])
            A("dve", lambda e: e.tensor_scalar(out=lamt[:, 0:1], in0=lamt[:, 0:1], scalar1=0.2, scalar2=None, op0=ALU.add),
              reads=["lam0"], writes=["lam0"])
            A("dve", lambda e: e.tensor_scalar(out=lamt[:, 1:2], in0=lamt[:, 0:1], scalar1=-1.0, scalar2=None, op0=ALU.mult),
              reads=["lam0"], writes=["lam1"])
            A("sp", lambda e: e.dma_start(out=sgl[:], in_=subln_g.partition_broadcast(128)), writes=["sgl"], dma=True)
            A("dve", lambda e: e.tensor_scalar(out=sg8[:], in0=sgl[:], scalar1=0.8, scalar2=None, op0=ALU.mult),
              reads=["sgl"], writes=["sg8"])
            A("sp", lambda e: e.dma_start(out=rbias[:], in_=router_bias.partition_broadcast(128)), writes=["rbias"], dma=True)
            A("sp", lambda e: e.dma_start(out=tab[:], in_=relb.rearrange("o (b h) -> (o b) h", h=12)), writes=["tab"], dma=True)
            A("sp", lambda e: e.dma_start(out=negt[:], in_=negc.rearrange("u s q -> s u q")), writes=["negt"], dma=True)
            for u in range(7):
                A("sp", lambda e, u=u: e.dma_start(out=cbt[:], in_=cbc[u]), writes=["cbt"], dma=True)
                for q in range(128):
                    A("pe", lambda e, q=q: e.matmul(pmb[:, q, 0:12], lhsT=cbt[:, q * 128:(q + 1) * 128], rhs=tab[:],
                                                    start=True, stop=True),
                      reads=["cbt", "tab"], writes=["pmb"])
                for qb_ in range(4):
                    A("dve", lambda e, u=u, qb_=qb_: e.tensor_tensor(
                        out=MB[:, u, :, qb_ * 32:(qb_ + 1) * 32],
                        in0=pmb[:, qb_ * 32:(qb_ + 1) * 32, 0:12].rearrange("s q h -> s h q"),
                        in1=negt[:, u, qb_ * 32:(qb_ + 1) * 32].unsqueeze(1).to_broadcast([128, 12, 32]), op=ALU.add),
                      reads=["pmb", "negt"], writes=["MB"])
            if os.environ.get("MK_DEBUG"):
                mbo = nc.dram_tensor("mbo", [128, 7 * 12 * 128], BF16, kind="ExternalOutput").ap()
                A("sp", lambda e: e.dma_start(out=mbo, in_=MB[:].rearrange("p a b c -> p (a b c)")), reads=["MB"], dma=True)
            S.emit()

        if STAGE >= 1:
            phase1_cache(nc, S, locals())
        if STAGE >= 2:
            phase2_proj(nc, S, locals())
        if STAGE >= 3:
            phase3_attn(nc, S, locals())
        mbes.close()
        if STAGE >= 4:
            phase4_moe(nc, S, locals())
    return nc


def phase1_cache(nc, S, G):
    A = S.add
    ident_b = G["ident_b"]
    cak, cav, cki, cbk, cbv = G["cak"], G["cav"], G["cki"], G["cbk"], G["cbv"]
    KAT, VA, KIT, KBT, VB = G["KAT"], G["VA"], G["KIT"], G["KBT"], G["VB"]
    with contextlib.ExitStack() as es:
        def sb(name, shape, dt):
            return es.enter_context(nc.sbuf_tensor(name, list(shape), dt))

        def ps(name, shape, dt=F32):
            return es.enter_context(nc.psum_tensor(name, list(shape), dt))

        kin = [sb("kin%d" % i, [128, 4, 1344], BF16) for i in range(2)]
        kout = [sb("kout%d" % i, [128, 11, 512], BF16) for i in range(2)]
        pt = [ps("pt%d" % i, [128, 2, 512], BF16) for i in range(4)]
        rk = Rot("kin", 2)
        ro = Rot("kout", 2)
        rp = Rot("pt", 4)
        for r in range(2):
            ctx = 1 + r
            A("pool", lambda e, r=r, ctx=ctx: e.dma_start(out=VA[ctx][0:PAST, :], in_=cav[r]), writes=[("VA", ctx)], dma=True)
            A("pool", lambda e, r=r, ctx=ctx: e.dma_start(out=VB[ctx][0:PAST, :], in_=cbv[r]), writes=[("VB", ctx)], dma=True)
            for blk in range(8):
                i, kk = rk.next()
                t0 = blk * 512
                A("pool", lambda e, i=i, r=r, t0=t0: e.dma_start(
                    out=kin[i][:, :, 0:256], in_=cak[r, t0:t0 + 512, :].rearrange("(j p) c -> p j c", p=128)),
                  writes=[kk], dma=True)
                A("pool", lambda e, i=i, r=r, t0=t0: e.dma_start(
                    out=kin[i][:, :, 256:320], in_=cki[r, t0:t0 + 512, :].rearrange("(j p) c -> p j c", p=128)),
                  writes=[kk], dma=True)
                A("pool", lambda e, i=i, r=r, t0=t0: e.dma_start(
                    out=kin[i][:, :, 320:1344], in_=cbk[r, t0:t0 + 512, :].rearrange("(j p) c -> p j c", p=128)),
                  writes=[kk], dma=True)
                o, ok = ro.next()
                cts = [(0, 128), (128, 128), (256, 64)] + [(320 + 128 * h, 128) for h in range(8)]
                for pair in range(6):
                    pi, pk = rp.next()
                    sub = cts[2 * pair:2 * pair + 2]
                    for si, (c0, rows) in enumerate(sub):
                        for j in range(4):
                            A("pe", lambda e, pi=pi, si=si, j=j, i=i, c0=c0, rows=rows: e.transpose(
                                pt[pi][0:rows, si, j * 128:(j + 1) * 128], kin[i][:, j, c0:c0 + rows], ident_b[:]),
                              reads=[kk, "ident_b"], writes=[pk])
                    if len(sub) == 2 and sub[1][1] == 128 and sub[0][1] == 128:
                        eng = "act" if pair % 2 == 0 else "dve"
                        if eng == "act":
                            A("act", lambda e, pi=pi, o=o, pair=pair: e.activation(
                                out=kout[o][:, 2 * pair:2 * pair + 2, :], in_=pt[pi][:, :, :], func=AF.Copy),
                              reads=[pk], writes=[ok])
                        else:
                            A("dve", lambda e, pi=pi, o=o, pair=pair: e.tensor_copy(
                                kout[o][:, 2 * pair:2 * pair + 2, :], pt[pi][:, :, :]),
                              reads=[pk], writes=[ok])
                    else:
                        for si, (c0, rows) in enumerate(sub):
                            A("dve", lambda e, pi=pi, o=o, pair=pair, si=si, rows=rows: e.tensor_copy(
                                kout[o][0:rows, 2 * pair + si, :], pt[pi][0:rows, si, :]),
                              reads=[pk], writes=[ok])
                A("sp", lambda e, o=o, ctx=ctx, t0=t0: e.dma_start(
                    out=KAT[ctx][:, :, t0:t0 + 512].rearrange("g d s -> d g s"), in_=kout[o][:, 0:2, :]),
                  reads=[ok], writes=[("KAT", ctx)], dma=True)
                A("sp", lambda e, o=o, ctx=ctx, t0=t0: e.dma_start(
                    out=KIT[ctx][:, t0:t0 + 512], in_=kout[o][0:64, 2, :]),
                  reads=[ok], writes=[("KIT", ctx)], dma=True)
                A("sp", lambda e, o=o, ctx=ctx, t0=t0: e.dma_start(
                    out=KBT[ctx][:, :, t0:t0 + 512].rearrange("g d s -> d g s"), in_=kout[o][:, 3:11, :]),
                  reads=[ok], writes=[("KBT", ctx)], dma=True)
        S.emit()


def phase2_proj(nc, S, G):
    A = S.add
    ident_b = G["ident_b"]
    AaT, BaT = G["AaT"], G["BaT"]
    w_in = G["w_in"]
    KAT, VA, KIT, KBT, VB = G["KAT"], G["VA"], G["KIT"], G["KBT"], G["VB"]
    QAT, QBT, QIT, IWS = G["QAT"], G["QBT"], G["QIT"], G["IWS"]
    xb, xown, xsmp = G["xb"], G["xown"], G["xsmp"]
    wv = w_in.rearrange("(kt p) n -> p kt n", p=128)
    SC = 128 ** -0.5
    with contextlib.ExitStack() as es:
        def sb(name, shape, dt):
            return es.enter_context(nc.sbuf_tensor(name, list(shape), dt))

        def ps(name, shape, dt=F32):
            return es.enter_context(nc.psum_tensor(name, list(shape), dt))

        hT = sb("hT", [128, 16, 2080], BF16)
        xt = [sb("xt%d" % i, [128, D], F32) for i in range(2)]
        xn = [sb("xn%d" % i, [128, D], BF16) for i in range(2)]
        junk = sb("junk", [128, D], BF16)
        ss = [sb("ss%d" % i, [128, 2], F32) for i in range(2)]
        tmpf = [sb("tmpf%d" % i, [128, 8, 128], F32) for i in range(2)]
        wbuf = [sb("wbuf%d" % i, [128, 16, 512], BF16) for i in range(2)]
        stb = [sb("stb%d" % i, [128, 512], BF16) for i in range(3)]
        stf = [sb("stf%d" % i, [128, 512], F32) for i in range(3)]
        pth = [ps("pth%d" % i, [128, 8, 128], BF16) for i in range(2)]
        pf = [ps("pf%d" % i, [128, 512]) for i in range(4)]
        rx, rpt, rtm, rw, rsb, rsf, rpf = (Rot("xt", 2), Rot("pth", 2), Rot("tmpf", 2), Rot("wbuf", 2), Rot("stb", 3),
                                           Rot("stf", 3), Rot("pf", 4))
        cnt = [0]

        def make_hT(xsrc, n, off, segs):
            i, xk = rx.next()
            A("sp", lambda e: e.dma_start(out=xt[i][0:n, :], in_=xsrc), writes=[xk], dma=True)
            A("act", lambda e: e.activation(out=junk[0:n, :], in_=xt[i][0:n, :], func=AF.Square, accum_out=ss[i][0:n, 0:1]),
              reads=[xk], writes=["junk", ("ss", i)])
            A("dve", lambda e: e.tensor_scalar(out=ss[i][0:n, 1:2], in0=ss[i][0:n, 0:1], scalar1=1.0 / D, scalar2=EPS,
                                               op0=ALU.mult, op1=ALU.add), reads=[("ss", i)], writes=[("rs", i)])
            A("act", lambda e: e.activation(out=ss[i][0:n, 1:2], in_=ss[i][0:n, 1:2], func=AF.Sqrt),
              reads=[("rs", i)], writes=[("rs", i)])
            A("dve", lambda e: e.reciprocal(out=ss[i][0:n, 1:2], in_=ss[i][0:n, 1:2]),
              reads=[("rs", i)], writes=[("rs", i)])
            A("dve", lambda e: e.tensor_scalar(out=xn[i][0:n, :], in0=xt[i][0:n, :], scalar1=ss[i][0:n, 1:2], scalar2=None,
                                               op0=ALU.mult), reads=[xk, ("rs", i)], writes=[("xn", i)])
            for half in range(2):
                pi, pk = rpt.next()
                for k8 in range(8):
                    kt = half * 8 + k8
                    A("pe", lambda e, pi=pi, k8=k8, kt=kt: e.transpose(
                        pth[pi][:, k8, 0:n], xn[i][0:n, kt * 128:(kt + 1) * 128], ident_b[0:n, 0:n]),
                      reads=[("xn", i), "ident_b"], writes=[pk])
                ti, tk = rtm.next()
                for (lo, hi, r) in segs:
                    w = hi - lo
                    A("dve", lambda e, pi=pi, ti=ti, lo=lo, hi=hi, r=r, w=w, half=half: e.tensor_tensor(
                        out=tmpf[ti][:, :, lo:hi], in0=pth[pi][:, :, lo:hi],
                        in1=AaT[:, r, half * 8:half * 8 + 8].unsqueeze(2).to_broadcast([128, 8, w]), op=ALU.mult),
                      reads=[pk, "AaT"], writes=[tk])
                    A("pool", lambda e, ti=ti, lo=lo, hi=hi, r=r, w=w, half=half: e.tensor_tensor(
                        out=hT[:, half * 8:half * 8 + 8, off + lo:off + hi], in0=tmpf[ti][:, :, lo:hi],
                        in1=BaT[:, r, half * 8:half * 8 + 8].unsqueeze(2).to_broadcast([128, 8, w]), op=ALU.add),
                      reads=[tk, "BaT"], writes=[("hT", off)])

        def load_w(c0, ncb):
            i, wk = rw.next()
            A("pool", lambda e: e.dma_start(out=wbuf[i][:, :, 0:ncb], in_=wv[:, :, c0:c0 + ncb]), writes=[wk], dma=True)
            return i, wk

        def evac(dst, src, scale, idx):
            if idx % 2 == 0:
                if scale == 1.0:
                    return ("act", lambda e: e.activation(out=dst, in_=src, func=AF.Copy))
                return ("act", lambda e: e.mul(dst, src, scale))
            if scale == 1.0:
                return ("dve", lambda e: e.tensor_copy(dst, src))
            return ("dve", lambda e: e.tensor_scalar(out=dst, in0=src, scalar1=scale, scalar2=None, op0=ALU.mult))

        def do_fm(i, wk, ncb, ct_base, chunks, scale, hkeys, outfn):
            for ct in range((ncb + 127) // 128):
                rows = min(128, ncb - ct * 128)
                for (t0, nt) in chunks:
                    pi, pk = rpf.next()
                    for kt in range(16):
                        A("pe", lambda e, pi=pi, kt=kt, ct=ct, rows=rows, t0=t0, nt=nt: e.matmul(
                            pf[pi][0:rows, 0:nt], lhsT=wbuf[i][:, kt, ct * 128:ct * 128 + rows], rhs=hT[:, kt, t0:t0 + nt],
                            start=(kt == 0), stop=(kt == 15)), reads=[wk] + hkeys, writes=[pk])
                    si, sk = rsb.next()
                    cnt[0] += 1
                    eng, fn = evac(stb[si][0:rows, 0:nt], pf[pi][0:rows, 0:nt], scale, cnt[0])
                    A(eng, fn, reads=[pk], writes=[sk])
                    outfn(stb[si], sk, rows, ct_base + ct, t0, nt)

        def do_tm(i, wk, ncb, cb0, tiles, scale, outfn):
            for (tix, off, n) in tiles:
                pi, pk = rpf.next()
                for kt in range(16):
                    A("pe", lambda e, pi=pi, kt=kt, off=off, n=n: e.matmul(
                        pf[pi][0:n, 0:ncb], lhsT=hT[:, kt, off:off + n], rhs=wbuf[i][:, kt, 0:ncb],
                        start=(kt == 0), stop=(kt == 15)), reads=[wk, ("hT", off)], writes=[pk])
                si, sk = rsf.next()
                cnt[0] += 1
                eng, fn = evac(stf[si][0:n, 0:ncb], pf[pi][0:n, 0:ncb], scale, cnt[0])
                A(eng, fn, reads=[pk], writes=[sk])
                outfn(stf[si], sk, tix, n, cb0, ncb)

        def dma_out(dst, src, sk, wkey, q="sp"):
            A(q, lambda e: e.dma_start(out=dst, in_=src), reads=[sk], writes=[wkey], dma=True)

        for ch in range(4):
            T0 = ch * 2048
            for t in range(16):
                make_hT(xb[T0 + t * 128:T0 + (t + 1) * 128, :], 128, t * 128, [(0, 128, 0)])
            hkeys = [("hT", t * 128) for t in range(16)]
            chunks = [(t0, 512) for t0 in range(0, 2048, 512)]
            tiles = [(t, t * 128, 128) for t in range(16)]

            def fm_out(dstT):
                def f(st, sk, rows, ctg, t0, nt):
                    dma_out(dstT[ctg][0:rows, T0 + t0:T0 + t0 + nt], st[0:rows, 0:nt], sk, ("scrK", id(dstT)))
                return f

            def tm_out(dstT):
                def f(st, sk, tix, n, cb0, ncb):
                    dma_out(dstT[T0 + tix * 128:T0 + tix * 128 + n, cb0:cb0 + ncb], st[0:n, 0:ncb], sk,
                            ("scrV", id(dstT)), q="pool")
                return f
            i, wk = load_w(1024, 256)
            do_fm(i, wk, 256, 0, chunks, 1.0, hkeys, fm_out(KAT[0]))
            i, wk = load_w(2560, 64)
            do_fm(i, wk, 64, 0, chunks, 1.0, hkeys, lambda st, sk, rows, ctg, t0, nt: dma_out(
                KIT[0][0:64, T0 + t0:T0 + t0 + nt], st[0:64, 0:nt], sk, "scrKI"))
            for cbk_ in range(2):
                i, wk = load_w(3664 + 512 * cbk_, 512)
                do_fm(i, wk, 512, 4 * cbk_, chunks, 1.0, hkeys, fm_out(KBT[0]))
            i, wk = load_w(1280, 256)
            do_tm(i, wk, 256, 0, tiles, 1.0, tm_out(VA[0]))
            for cbv_ in range(2):
                i, wk = load_w(4688 + 512 * cbv_, 512)
                do_tm(i, wk, 512, 512 * cbv_, tiles, 1.0, tm_out(VB[0]))

        for t in range(16):
            make_hT(xown[t * 128:(t + 1) * 128, :], 128, t * 128, [(0, 128, 0)])
        make_hT(xsmp, 32, 2048, [(0, 16, 1), (16, 32, 2)])
        hkeys = [("hT", t * 128) for t in range(17)]
        chunks = [(t0, 512) for t0 in range(0, 2048, 512)] + [(2048, 32)]
        tiles = [(t, t * 128, 128) for t in range(16)] + [(16, 2048, 32)]
        schunk = [(2048, 32)]
        G["p2"] = dict(ak=(G["ak_own"], G["ak_s"]), av=(G["av_own"], G["av_s"]), ki=(G["ki_own"], G["ki_s"]),
                       bk=(G["bk_own"], G["bk_s"]), bv=(G["bv_own"], G["bv_s"]))

        def q_out(dstT):
            def f(st, sk, rows, ctg, t0, nt):
                dma_out(dstT[ctg][0:rows, t0:t0 + nt], st[0:rows, 0:nt], sk, ("scrQ", id(dstT)))
            return f

        def rows_out(name, vscr=None):
            own, smp = G["p2"][name]

            def f(st, sk, tix, n, cb0, ncb):
                if tix < 16:
                    dma_out(own[tix * 128:(tix + 1) * 128, cb0:cb0 + ncb], st[0:n, 0:ncb], sk, ("out", name))
                else:
                    dma_out(smp[:, cb0:cb0 + ncb], st[0:n, 0:ncb], sk, ("out", name))
                    if vscr is not None:
                        for r in range(2):
                            dma_out(vscr[1 + r][PAST:PAST + 16, cb0:cb0 + ncb], st[16 * r:16 * r + 16, 0:ncb], sk,
                                    ("scrVs", name, r), q="pool")
            return f

        def sk_out(dstT):
            def f(st, sk, rows, ctg, t0, nt):
                for r in range(2):
                    dma_out(dstT[1 + r][ctg][0:rows, PAST:PAST + 16], st[0:rows, 16 * r:16 * r + 16], sk, ("scrKs", id(dstT), r))
            return f
        for cb in range(2):
            i, wk = load_w(512 * cb, 512)
            do_fm(i, wk, 512, 4 * cb, chunks, SC, hkeys, q_out(QAT))
        i, wk = load_w(1024, 256)
        do_tm(i, wk, 256, 0, tiles, 1.0, rows_out("ak"))
        do_fm(i, wk, 256, 0, schunk, 1.0, hkeys, lambda st, sk, rows, ctg, t0, nt: [dma_out(
            KAT[1 + r][ctg][0:rows, PAST:PAST + 16], st[0:rows, 16 * r:16 * r + 16], sk, ("scrKs", "a", r)) for r in range(2)])
        i, wk = load_w(1280, 256)
        do_tm(i, wk, 256, 0, tiles, 1.0, rows_out("av", VA))
        for cb in range(2):
            i, wk = load_w(1536 + 512 * cb, 512)
            do_fm(i, wk, 512, 4 * cb, chunks, 0.125, hkeys, q_out(QIT))
        i, wk = load_w(2560, 64)
        do_tm(i, wk, 64, 0, tiles, 1.0, rows_out("ki"))
        do_fm(i, wk, 64, 0, schunk, 1.0, hkeys, lambda st, sk, rows, ctg, t0, nt: [dma_out(
            KIT[1 + r][0:64, PAST:PAST + 16], st[0:64, 16 * r:16 * r + 16], sk, ("scrKs", "i", r)) for r in range(2)])
        i, wk = load_w(2624, 16)
        do_tm(i, wk, 16, 0, tiles, 0.25, lambda st, sk, tix, n, cb0, ncb: dma_out(
            IWS[tix * 128:tix * 128 + n, :], st[0:n, 0:16], sk, "scrIW"))
        for cb in range(2):
            i, wk = load_w(2640 + 512 * cb, 512)
            do_fm(i, wk, 512, 4 * cb, chunks, SC, hkeys, q_out(QBT))
        for cb in range(2):
            i, wk = load_w(3664 + 512 * cb, 512)
            do_tm(i, wk, 512, 512 * cb, tiles, 1.0, rows_out("bk"))
            do_fm(i, wk, 512, 4 * cb, schunk, 1.0, hkeys, lambda st, sk, rows, ctg, t0, nt: [dma_out(
                KBT[1 + r][ctg][0:rows, PAST:PAST + 16], st[0:rows, 16 * r:16 * r + 16], sk, ("scrKs", "b", r)) for r in range(2)])
        for cb in range(2):
            i, wk = load_w(4688 + 512 * cb, 512)
            do_tm(i, wk, 512, 512 * cb, tiles, 1.0, rows_out("bv", VB))
        S.emit()


def phase3_attn(nc, S, G):
    A = S.add
    ident_b, ones_b, MB, lamt, sg8 = G["ident_b"], G["ones_b"], G["MB"], G["lamt"], G["sg8"]
    KAT, VA, KIT, KBT, VB = G["KAT"], G["VA"], G["KIT"], G["KBT"], G["VB"]
    QAT, QBT, QIT, IWS, mixS = G["QAT"], G["QBT"], G["QIT"], G["IWS"], G["mixS"]
    vis01, visneg = G["vis01"], G["visneg"]
    with contextlib.ExitStack() as es:
        def sb(name, shape, dt):
            return es.enter_context(nc.sbuf_tensor(name, list(shape), dt))

        def ps(name, shape, dt=F32):
            return es.enter_context(nc.psum_tensor(name, list(shape), dt))

        score = sb("score", [128, SEQ], F32)
        maskb = sb("maskb", [128, SEQ], BF16)
        v01 = sb("v01", [128, 512], F32)
        vng = sb("vng", [128, 512], F32)
        kilo = [sb("kilo%d" % i, [128, 512], BF16) for i in range(2)]
        kihi = [sb("kihi%d" % i, [128, 512], BF16) for i in range(2)]
        rbuf = [sb("rbuf%d" % i, [128, 512], BF16) for i in range(3)]
        kab = [sb("kab%d" % i, [128, 2, 512], BF16) for i in range(2)]
        vab = [sb("vab%d" % i, [128, 4, 256], BF16) for i in range(2)]
        kbb = [sb("kbb%d" % i, [128, 8, 512], BF16) for i in range(2)]
        vbb = [sb("vbb%d" % i, [128, 4, 1024], BF16) for i in range(2)]
        qa = [sb("qa%d" % i, [128, 8, 128], BF16) for i in range(2)]
        qb = [sb("qb%d" % i, [128, 8, 128], BF16) for i in range(2)]
        qi = [sb("qi%d" % i, [128, 8, 128], BF16) for i in range(2)]
        iw = [sb("iw%d" % i, [128, 16], F32) for i in range(2)]
        pbuf = [sb("pbuf%d" % i, [128, 4, 128], BF16) for i in range(4)]
        mix = [sb("mix%d" % i, [128, D], BF16) for i in range(2)]
        ob = sb("ob", [128, 4, 256], F32)
        junk = sb("junk3", [128, 256], BF16)
        bis = sb("bis", [128, 8], F32)
        zr = sb("zr", [128, 24], F32)
        psS = [ps("psS%d" % i, [128, 4, 128]) for i in range(3)]
        psO = [ps("psO%d" % i, [128, 512]) for i in range(4)]
        psZ = ps("psZ", [128, 16])
        rki, rrb, rka, rkb, rpb, rpS = Rot("ki", 2), Rot("rbuf", 3), Rot("ka", 2), Rot("kb", 2), Rot("pbuf", 4), Rot("psS", 3)

        A("sp", lambda e: e.dma_start(out=v01[:], in_=vis01), writes=["v01"], dma=True)
        A("sp", lambda e: e.dma_start(out=vng[:], in_=visneg), writes=["vng"], dma=True)
        for i in range(2):
            A("pool", lambda e, i=i: e.memset(kilo[i][:], 0.0), writes=[("ki", i)])
            A("pool", lambda e, i=i: e.memset(kihi[i][:], 0.0), writes=[("ki", i)])

        for n in range(18):
            if n < 16:
                m, ctx, nq, NT, tok0, lastk = n, 0, 128, 4 * n + 4, n * 128, 128
                slot_of = lambda t, m=m: (t - (4 * m - 1)) if t >= 4 * m - 1 else None
            else:
                r = n - 16
                m, ctx, nq, NT, tok0, lastk = None, 1 + r, 16, 33, 2048 + 16 * r, 16
                slot_of = lambda t: {31: 5, 32: 6}.get(t)
            L = (NT - 1) * 128 + lastk
            qs = n % 2
            qk = ("q", qs)
            A("sp", lambda e, qs=qs, tok0=tok0, nq=nq: e.dma_start(
                out=qa[qs][:, :, 0:nq], in_=QAT[:, :, tok0:tok0 + nq].rearrange("h d q -> d h q")),
              reads=[("scrQ", id(QAT))], writes=[qk], dma=True)
            A("sp", lambda e, qs=qs, tok0=tok0, nq=nq: e.dma_start(
                out=qb[qs][:, :, 0:nq], in_=QBT[:, :, tok0:tok0 + nq].rearrange("h d q -> d h q")),
              writes=[qk], dma=True)
            A("sp", lambda e, qs=qs, tok0=tok0, nq=nq: e.dma_start(
                out=qi[qs][:, :, 0:nq], in_=QIT[:, :, tok0:tok0 + nq].rearrange("h d q -> d h q")),
              writes=[qk], dma=True)
            A("sp", lambda e, qs=qs, tok0=tok0, nq=nq: e.dma_start(out=iw[qs][0:nq, :], in_=IWS[tok0:tok0 + nq, :]),
              writes=[qk], dma=True)
            blocks = []
            for k0 in range(0, L, 512):
                blocks.append((k0, min(512, L - k0)))
            for (k0, nk) in blocks:
                i, kk = rki.next()
                A("sp", lambda e, i=i, k0=k0, nk=nk, ctx=ctx: e.dma_start(out=kilo[i][0:64, 0:nk], in_=KIT[ctx][:, k0:k0 + nk]),
                  writes=[kk], dma=True)
                A("sp", lambda e, i=i, k0=k0, nk=nk, ctx=ctx: e.dma_start(out=kihi[i][64:128, 0:nk], in_=KIT[ctx][:, k0:k0 + nk]),
                  writes=[kk], dma=True)
                for h in range(16):
                    pi, pk = rpS.next()
                    src = kilo if h % 2 == 0 else kihi
                    pflat = psS[pi][:].rearrange("p a b -> p (a b)")
                    A("pe", lambda e, pflat=pflat, src=src, i=i, h=h, nk=nk, qs=qs, nq=nq: e.matmul(
                        pflat[0:nq, 0:nk], lhsT=qi[qs][:, h // 2, 0:nq], rhs=src[i][:, 0:nk], start=True, stop=True),
                      reads=[qk, kk], writes=[pk])
                    ri, rk = rrb.next()
                    A("act", lambda e, pflat=pflat, ri=ri, nk=nk, nq=nq: e.activation(
                        out=rbuf[ri][0:nq, 0:nk], in_=pflat[0:nq, 0:nk], func=AF.Relu), reads=[pk], writes=[rk])
                    if h == 0:
                        A("dve", lambda e, ri=ri, k0=k0, nk=nk, nq=nq, qs=qs: e.tensor_scalar(
                            out=score[0:nq, k0:k0 + nk], in0=rbuf[ri][0:nq, 0:nk], scalar1=iw[qs][0:nq, 0:1], scalar2=None,
                            op0=ALU.mult), reads=[rk, qk], writes=["score"])
                    else:
                        A("dve", lambda e, ri=ri, k0=k0, nk=nk, nq=nq, qs=qs, h=h: e.scalar_tensor_tensor(
                            out=score[0:nq, k0:k0 + nk], in0=rbuf[ri][0:nq, 0:nk], scalar=iw[qs][0:nq, h:h + 1],
                            in1=score[0:nq, k0:k0 + nk], op0=ALU.mult, op1=ALU.add), reads=[rk, qk, "score"], writes=["score"])
            if n < 16:
                k0 = 4 * m * 128
                A("dve", lambda e, k0=k0: e.tensor_tensor(out=score[:, k0:k0 + 512], in0=score[:, k0:k0 + 512], in1=v01[:],
                                                          op=ALU.mult), reads=["score", "v01"], writes=["score"])
            A("dve", lambda e, nq=nq, L=L: e.tensor_reduce(out=bis[0:nq, 0:1], in_=score[0:nq, 0:L], axis=AX.X, op=ALU.max,
                                                           apply_absolute_value=True), reads=["score"], writes=["bisA"])
            if n < 16:
                A("dve", lambda e, k0=k0: e.tensor_tensor(out=score[:, k0:k0 + 512], in0=score[:, k0:k0 + 512], in1=vng[:],
                                                          op=ALU.add), reads=["score", "vng", "bisA"], writes=["score"])
            A("dve", lambda e, nq=nq: e.tensor_scalar(out=bis[0:nq, 1:2], in0=bis[0:nq, 0:1], scalar1=2.0, scalar2=None,
                                                      op0=ALU.mult), reads=["bisA"], writes=["bisR"])
            A("dve", lambda e, nq=nq: e.tensor_scalar(out=bis[0:nq, 2:3], in0=bis[0:nq, 0:1], scalar1=-1.0, scalar2=None,
                                                      op0=ALU.mult), reads=["bisA"], writes=["bisT"])
            for it in range(NBIS):
                c = 2.0 ** -(it + 1)
                A("dve", lambda e, nq=nq, c=c: e.scalar_tensor_tensor(
                    out=bis[0:nq, 3:4], in0=bis[0:nq, 1:2], scalar=c, in1=bis[0:nq, 2:3], op0=ALU.mult, op1=ALU.add),
                  reads=["bisR", "bisT"], writes=["bisC"])
                A("dve", lambda e, nq=nq, L=L: e.tensor_scalar(
                    out=maskb[0:nq, 0:L], in0=score[0:nq, 0:L], scalar1=bis[0:nq, 3:4], scalar2=None, op0=ALU.is_ge,
                    op1=ALU.add, accum_out=bis[0:nq, 4:5]), reads=["score", "bisC"], writes=["maskb", "bisN"])
                A("dve", lambda e, nq=nq, c=c: e.tensor_scalar(
                    out=bis[0:nq, 5:6], in0=bis[0:nq, 4:5], scalar1=255.5, scalar2=c, op0=ALU.is_ge, op1=ALU.mult),
                  reads=["bisN"], writes=["bisM"])
                A("dve", lambda e, nq=nq: e.scalar_tensor_tensor(
                    out=bis[0:nq, 2:3], in0=bis[0:nq, 5:6], scalar=bis[0:nq, 1:2], in1=bis[0:nq, 2:3], op0=ALU.mult,
                    op1=ALU.add), reads=["bisM", "bisR", "bisT"], writes=["bisT"])
            A("dve", lambda e, nq=nq, L=L: e.tensor_scalar(
                out=maskb[0:nq, 0:L], in0=score[0:nq, 0:L], scalar1=bis[0:nq, 2:3], scalar2=NEGV, op0=ALU.is_lt, op1=ALU.mult),
              reads=["score", "bisT"], writes=["maskb"])
            tiles = [(t, 128 if t < NT - 1 else lastk) for t in range(NT)]
            if n < 16 and m == 0:
                pass
            t_first, t_last = 0, NT - 1
            psOA = [psO[b][:].rearrange("p (a c) -> p a c", a=4) for b in range(2)]
            psOB = [psO[b][:].rearrange("p (a c) -> p a c", a=2) for b in range(4)]
            for (k0, nk) in blocks:
                i, kk = rka.next()
                A("sp", lambda e, i=i, k0=k0, nk=nk, ctx=ctx: e.dma_start(
                    out=kab[i][:, :, 0:nk], in_=KAT[ctx][:, :, k0:k0 + nk].rearrange("g d s -> d g s")), writes=[kk], dma=True)
                if nk == 512:
                    A("sp", lambda e, i=i, k0=k0, ctx=ctx: e.dma_start(
                        out=vab[i][:], in_=VA[ctx][k0:k0 + 512, :].rearrange("(j p) c -> p j c", p=128)), writes=[kk], dma=True)
                else:
                    A("sp", lambda e, i=i, k0=k0, nk=nk, ctx=ctx: e.dma_start(
                        out=vab[i][0:nk, 0, :], in_=VA[ctx][k0:k0 + nk, :]), writes=[kk], dma=True)
                for j in range((nk + 127) // 128):
                    t = k0 // 128 + j
                    ns = min(128, nk - j * 128)
                    slot = slot_of(t)
                    for g in range(2):
                        pi, pk = rpS.next()
                        A("pe", lambda e, pi=pi, i=i, g=g, j=j, ns=ns, qs=qs, nq=nq: e.matmul(
                            psS[pi][0:ns, :, 0:nq], lhsT=kab[i][:, g, j * 128:j * 128 + ns], rhs=qa[qs][:, 4 * g:4 * g + 4, 0:nq],
                            start=True, stop=False), reads=[kk, qk], writes=[pk])
                        A("pe", lambda e, pi=pi, t=t, ns=ns, nq=nq, slot=slot: e.matmul(
                            psS[pi][0:ns, :, 0:nq], lhsT=maskb[0:nq, t * 128:t * 128 + ns],
                            rhs=ident_b[0:nq, 0:nq].unsqueeze(1).to_broadcast([nq, 4, nq]),
                            start=False, stop=(slot is None)), reads=["maskb", "ident_b"], writes=[pk])
                        if slot is not None:
                            A("pe", lambda e, pi=pi, ns=ns, nq=nq, slot=slot, g=g: e.matmul(
                                psS[pi][0:ns, :, 0:nq], lhsT=ident_b[0:ns, 0:ns], rhs=MB[0:ns, slot, 4 * g:4 * g + 4, 0:nq],
                                start=False, stop=True), reads=["MB", "ident_b"], writes=[pk])
                        bi, bk = rpb.next()
                        A("act", lambda e, pi=pi, bi=bi, ns=ns, nq=nq: e.activation(
                            out=pbuf[bi][0:ns, :, 0:nq], in_=psS[pi][0:ns, :, 0:nq], func=AF.Exp), reads=[pk], writes=[bk])
                        for hh in range(4):
                            h = 4 * g + hh
                            A("pe", lambda e, bi=bi, hh=hh, h=h, ns=ns, nq=nq, i=i, j=j, g=g, t=t: e.matmul(
                                psOA[h // 4][0:nq, h % 4, :], lhsT=pbuf[bi][0:ns, hh, 0:nq], rhs=vab[i][0:ns, j, g * 128:(g + 1) * 128],
                                start=(t == t_first and hh == 0), stop=(t == t_last)), reads=[bk, kk], writes=[("psO", h // 4)])
                            A("pe", lambda e, bi=bi, hh=hh, h=h, ns=ns, nq=nq, t=t: e.matmul(
                                psZ[0:nq, h:h + 1], lhsT=pbuf[bi][0:ns, hh, 0:nq], rhs=ones_b[0:ns, 0:1],
                                start=(t == t_first and h == 0), stop=(t == t_last)), reads=[bk, "ones_b"], writes=["psZ"])
            mi = n % 2
            mk = ("mix", mi)
            A("dve", lambda e, nq=nq: e.reciprocal(out=zr[0:nq, 0:8], in_=psZ[0:nq, 0:8]), reads=["psZ"], writes=["zrA"])
            for h in range(8):
                if h % 2 == 0:
                    A("dve", lambda e, h=h, nq=nq, mi=mi: e.tensor_scalar(
                        out=mix[mi][0:nq, h * 128:(h + 1) * 128], in0=psOA[h // 4][0:nq, h % 4, :], scalar1=zr[0:nq, h:h + 1],
                        scalar2=None, op0=ALU.mult), reads=[("psO", h // 4), "zrA"], writes=[mk])
                else:
                    A("act", lambda e, h=h, nq=nq, mi=mi: e.activation(
                        out=mix[mi][0:nq, h * 128:(h + 1) * 128], in_=psOA[h // 4][0:nq, h % 4, :], func=AF.Copy,
                        scale=zr[0:nq, h:h + 1]), reads=[("psO", h // 4), "zrA"], writes=[mk])
            for (k0, nk) in blocks:
                i, kk = rkb.next()
                A("sp", lambda e, i=i, k0=k0, nk=nk, ctx=ctx: e.dma_start(
                    out=kbb[i][:, :, 0:nk], in_=KBT[ctx][:, :, k0:k0 + nk].rearrange("g d s -> d g s")), writes=[kk], dma=True)
                if nk == 512:
                    A("sp", lambda e, i=i, k0=k0, ctx=ctx: e.dma_start(
                        out=vbb[i][:], in_=VB[ctx][k0:k0 + 512, :].rearrange("(j p) c -> p j c", p=128)), writes=[kk], dma=True)
                else:
                    A("sp", lambda e, i=i, k0=k0, nk=nk, ctx=ctx: e.dma_start(
                        out=vbb[i][0:nk, 0, :], in_=VB[ctx][k0:k0 + nk, :]), writes=[kk], dma=True)
                for j in range((nk + 127) // 128):
                    t = k0 // 128 + j
                    ns = min(128, nk - j * 128)
                    slot = slot_of(t)
                    for half in range(2):
                        pi, pk = rpS.next()
                        for q4 in range(4):
                            hc = half * 4 + q4
                            h = hc // 2
                            A("pe", lambda e, pi=pi, i=i, hc=hc, q4=q4, j=j, ns=ns, qs=qs, nq=nq, slot=slot: e.matmul(
                                psS[pi][0:ns, q4, 0:nq], lhsT=kbb[i][:, hc, j * 128:j * 128 + ns], rhs=qb[qs][:, hc, 0:nq],
                                start=True, stop=(slot is None)), reads=[kk, qk], writes=[pk])
                            if slot is not None:
                                A("pe", lambda e, pi=pi, q4=q4, ns=ns, nq=nq, slot=slot, h=h: e.matmul(
                                    psS[pi][0:ns, q4, 0:nq], lhsT=ident_b[0:ns, 0:ns], rhs=MB[0:ns, slot, 8 + h, 0:nq],
                                    start=False, stop=True), reads=["MB", "ident_b"], writes=[pk])
                        bi, bk = rpb.next()
                        A("act", lambda e, pi=pi, bi=bi, ns=ns, nq=nq: e.activation(
                            out=pbuf[bi][0:ns, :, 0:nq], in_=psS[pi][0:ns, :, 0:nq], func=AF.Exp), reads=[pk], writes=[bk])
                        for q4 in range(4):
                            hc = half * 4 + q4
                            h = hc // 2
                            A("pe", lambda e, bi=bi, q4=q4, hc=hc, h=h, ns=ns, nq=nq, i=i, j=j, t=t: e.matmul(
                                psOB[hc // 2][0:nq, hc % 2, :], lhsT=pbuf[bi][0:ns, q4, 0:nq], rhs=vbb[i][0:ns, j, h * 256:(h + 1) * 256],
                                start=(t == t_first and hc % 2 == 0), stop=(t == t_last)), reads=[bk, kk], writes=[("psO", hc // 2)])
                            A("pe", lambda e, bi=bi, q4=q4, hc=hc, ns=ns, nq=nq, t=t: e.matmul(
                                psZ[0:nq, 8 + hc:9 + hc], lhsT=pbuf[bi][0:ns, q4, 0:nq], rhs=ones_b[0:ns, 0:1],
                                start=(t == t_first and hc == 0), stop=(t == t_last)), reads=[bk, "ones_b"], writes=["psZ"])
            A("dve", lambda e, nq=nq: e.reciprocal(out=zr[0:nq, 8:16], in_=psZ[0:nq, 8:16]), reads=["psZ"], writes=["zrB"])
            for h in range(4):
                A("dve", lambda e, h=h, nq=nq: e.tensor_scalar(
                    out=zr[0:nq, 16 + h:17 + h], in0=zr[0:nq, 9 + 2 * h:10 + 2 * h], scalar1=lamt[0:nq, 1:2], scalar2=None,
                    op0=ALU.mult), reads=["zrB"], writes=["zrL"])
                A("dve", lambda e, h=h, nq=nq: e.tensor_scalar(
                    out=ob[0:nq, h, :], in0=psOB[h][0:nq, 0, :], scalar1=zr[0:nq, 8 + 2 * h:9 + 2 * h], scalar2=None,
                    op0=ALU.mult), reads=[("psO", h), "zrB"], writes=[("ob", h)])
                A("dve", lambda e, h=h, nq=nq: e.scalar_tensor_tensor(
                    out=ob[0:nq, h, :], in0=psOB[h][0:nq, 1, :], scalar=zr[0:nq, 16 + h:17 + h], in1=ob[0:nq, h, :],
                    op0=ALU.mult, op1=ALU.add), reads=[("psO", h), "zrL", ("ob", h)], writes=[("ob", h)])
                A("act", lambda e, h=h, nq=nq: e.activation(
                    out=junk[0:nq, :], in_=ob[0:nq, h, :], func=AF.Square, accum_out=zr[0:nq, 20 + h:21 + h]),
                  reads=[("ob", h)], writes=["junk3", "zrS"])
            A("dve", lambda e, nq=nq: e.tensor_scalar(out=zr[0:nq, 20:24], in0=zr[0:nq, 20:24], scalar1=1.0 / 256, scalar2=EPS,
                                                      op0=ALU.mult, op1=ALU.add), reads=["zrS"], writes=["zrS"])
            A("act", lambda e, nq=nq: e.activation(out=zr[0:nq, 20:24], in_=zr[0:nq, 20:24], func=AF.Sqrt),
              reads=["zrS"], writes=["zrS"])
            A("dve", lambda e, nq=nq: e.reciprocal(out=zr[0:nq, 20:24], in_=zr[0:nq, 20:24]), reads=["zrS"], writes=["zrS"])
            for h in range(4):
                A("dve", lambda e, h=h, nq=nq, mi=mi: e.scalar_tensor_tensor(
                    out=mix[mi][0:nq, 1024 + h * 256:1024 + (h + 1) * 256], in0=ob[0:nq, h, :], scalar=zr[0:nq, 20 + h:21 + h],
                    in1=sg8[0:nq, :], op0=ALU.mult, op1=ALU.mult), reads=[("ob", h), "zrS", "sg8"], writes=[mk])
            A("sp", lambda e, mi=mi, nq=nq, tok0=tok0: e.dma_start(out=mixS[tok0:tok0 + nq, :], in_=mix[mi][0:nq, :]),
              reads=[mk], writes=["mixS"], dma=True)
        S.emit()


def phase4_moe(nc, S, G):
    A = S.add
    ident_b, ident_f = G["ident_b"], G["ident_f"]
    AfT, BfT, rbias = G["AfT"], G["BfT"], G["rbias"]
    modS, mixS, x1S = G["modS"], G["mixS"], G["x1S"]
    xown, xsmp = G["xown"], G["xsmp"]
    w_out, w_router = G["w_out"], G["w_router"]
    w_gate, w_up, w_down = G["w_gate"], G["w_up"], G["w_down"]
    ws_gate, ws_up, ws_down, final_g = G["ws_gate"], G["ws_up"], G["ws_down"], G["final_g"]
    y_own, y_smp = G["y_own"], G["y_smp"]
    BIG = 1.0e9
    for hf in range(2):
        tiles = []
        for tl in range(8):
            n = hf * 8 + tl
            tiles.append((tl, 128, tl * 128, n * 128, xown[n * 128:(n + 1) * 128, :], [(0, 128, 0)]))
        if hf == 1:
            tiles.append((8, 32, 1024, 2048, xsmp, [(0, 16, 1), (16, 32, 2)]))
        TH = 1024 + (32 if hf == 1 else 0)
        blocks = [(0, 512), (512, 512)] + ([(1024, 32)] if hf == 1 else [])
        with contextlib.ExitStack() as oes:
            h2T = oes.enter_context(nc.sbuf_tensor("h2T_h%d" % hf, [128, 16, 1056], BF16))
            gatesT = oes.enter_context(nc.sbuf_tensor("gatesT_h%d" % hf, [65, 1056], F32))
            with contextlib.ExitStack() as es:
                def sb(name, shape, dt):
                    return es.enter_context(nc.sbuf_tensor(name + "_h%d" % hf, list(shape), dt))

                def ps(name, shape, dt=F32):
                    return es.enter_context(nc.psum_tensor(name + "_h%d" % hf, list(shape), dt))

                wo = sb("wo", [128, 16, D], BF16)
                wr = sb("wr", [128, 16, NEXP], BF16)
                gab = [sb("gab%d" % i, [128, D], F32) for i in range(2)]
                mixt = [sb("mixt%d" % i, [128, D], BF16) for i in range(2)]
                mixT = [sb("mixT%d" % i, [128, 16, 128], BF16) for i in range(2)]
                xt = [sb("xt4%d" % i, [128, D], F32) for i in range(1)]
                x1 = [sb("x1_%d" % i, [128, D], F32) for i in range(1)]
                xn = [sb("xn4%d" % i, [128, D], BF16) for i in range(2)]
                junk = sb("junk4", [128, D], BF16)
                tmpf = [sb("tmpf4%d" % i, [128, 8, 128], F32) for i in range(2)]
                ss = [sb("ss4%d" % i, [128, 2], F32) for i in range(2)]
                rt = [sb("rt%d" % i, [128, 6, NEXP], F32) for i in range(2)]
                rs_ = [sb("rs%d" % i, [128, 48], F32) for i in range(2)]
                pth = [ps("pth4%d" % i, [128, 8, 128], BF16) for i in range(2)]
                py = [ps("py%d" % i, [128, 512]) for i in range(3)]
                pr = ps("pr", [128, NEXP])
                pg = ps("pg", [128, 128])
                rpt, rpy, rtm = Rot("pth4", 2), Rot("py", 3), Rot("tmpf4", 2)
                wov = w_out.rearrange("(ft p) d -> p ft d", p=128)
                for c in range(4):
                    A("pool", lambda e, c=c: e.dma_start(out=wo[:, 4 * c:4 * c + 4, :], in_=wov[:, 4 * c:4 * c + 4, :]),
                      writes=[("wo", c)], dma=True)
                A("pool", lambda e: e.dma_start(out=wr[:], in_=w_router.rearrange("(p kt) n -> p kt n", kt=16)),
                  writes=["wr"], dma=True)
                A("sp", lambda e: e.dma_start(out=gab[0][:], in_=modS[0:1, 4096:6144].partition_broadcast(128)),
                  writes=[("gab", 0)], dma=True)
                if hf == 1:
                    for r in range(2):
                        A("sp", lambda e, r=r: e.dma_start(out=gab[1][16 * r:16 * r + 16, :],
                                                           in_=modS[1 + r:2 + r, 4096:6144].partition_broadcast(16)),
                          writes=[("gab", 1)], dma=True)
                A("pool", lambda e: e.memset(gatesT[64:65, :], 1.0), writes=["gatesT"])
                for (tl, nq, off, tok0, xsrc, segs) in tiles:
                    i = tl % 2
                    gi = 1 if nq == 32 else 0
                    A("sp", lambda e, i=i, nq=nq, tok0=tok0: e.dma_start(out=mixt[0][0:nq, :], in_=mixS[tok0:tok0 + nq, :]),
                      reads=["mixS"], writes=[("mixt", i)], dma=True)
                    A("sp", lambda e, i=i, nq=nq, xsrc=xsrc: e.dma_start(out=xt[0][0:nq, :], in_=xsrc), writes=[("xt", 0)], dma=True)
                    for half in range(2):
                        pi, pk = rpt.next()
                        for k8 in range(8):
                            ft = half * 8 + k8
                            A("pe", lambda e, pi=pi, k8=k8, ft=ft, i=i, nq=nq: e.transpose(
                                pth[pi][:, k8, 0:nq], mixt[0][0:nq, ft * 128:(ft + 1) * 128], ident_b[0:nq, 0:nq]),
                              reads=[("mixt", i), "ident_b"], writes=[pk])
                        if half == 0:
                            A("act", lambda e, pi=pi, i=i, nq=nq: e.activation(out=mixT[i][:, 0:8, 0:nq], in_=pth[pi][:, :, 0:nq],
                                                                               func=AF.Copy), reads=[pk], writes=[("mixT", i)])
                        else:
                            A("dve", lambda e, pi=pi, i=i, nq=nq: e.tensor_copy(mixT[i][:, 8:16, 0:nq], pth[pi][:, :, 0:nq]),
                              reads=[pk], writes=[("mixT", i)])
                    for db in range(4):
                        yi, yk = rpy.next()
                        for ft in range(16):
                            A("pe", lambda e, yi=yi, ft=ft, db=db, i=i, nq=nq: e.matmul(
                                py[yi][0:nq, :], lhsT=mixT[i][:, ft, 0:nq], rhs=wo[:, ft, db * 512:(db + 1) * 512],
                                start=(ft == 0), stop=(ft == 15)), reads=[("mixT", i), ("wo", ft // 4)], writes=[yk])
                        A("dve", lambda e, yi=yi, db=db, i=i, nq=nq, gi=gi: e.tensor_tensor(
                            out=x1[0][0:nq, db * 512:(db + 1) * 512], in0=py[yi][0:nq, :], in1=gab[gi][0:nq, db * 512:(db + 1) * 512],
                            op=ALU.mult), reads=[yk, ("gab", gi)], writes=[("x1", 0, db)])
                        A("pool", lambda e, db=db, i=i, nq=nq: e.tensor_tensor(
                            out=x1[0][0:nq, db * 512:(db + 1) * 512], in0=x1[0][0:nq, db * 512:(db + 1) * 512],
                            in1=xt[0][0:nq, db * 512:(db + 1) * 512], op=ALU.add),
                          reads=[("x1", 0, db), ("xt", 0)], writes=[("x1", 0, db)])
                    x1k = [("x1", 0, db) for db in range(4)]
                    A("sp", lambda e, i=i, nq=nq, tok0=tok0: e.dma_start(out=x1S[tok0:tok0 + nq, :], in_=x1[0][0:nq, :]),
                      reads=x1k, writes=["x1S"], dma=True)
                    A("act", lambda e, i=i, nq=nq: e.activation(out=junk[0:nq, :], in_=x1[0][0:nq, :], func=AF.Square,
                                                                accum_out=ss[i][0:nq, 0:1]), reads=x1k, writes=["junk4", ("ss4", i)])
                    A("dve", lambda e, i=i, nq=nq: e.tensor_scalar(out=ss[i][0:nq, 1:2], in0=ss[i][0:nq, 0:1], scalar1=1.0 / D,
                                                                   scalar2=EPS, op0=ALU.mult, op1=ALU.add),
                      reads=[("ss4", i)], writes=[("rs4", i)])
                    A("act", lambda e, i=i, nq=nq: e.activation(out=ss[i][0:nq, 1:2], in_=ss[i][0:nq, 1:2], func=AF.Sqrt),
                      reads=[("rs4", i)], writes=[("rs4", i)])
                    A("dve", lambda e, i=i, nq=nq: e.reciprocal(out=ss[i][0:nq, 1:2], in_=ss[i][0:nq, 1:2]),
                      reads=[("rs4", i)], writes=[("rs4", i)])
                    A("dve", lambda e, i=i, nq=nq: e.tensor_scalar(
                        out=xn[i][0:nq, :].rearrange("n (kt p) -> n kt p", kt=16),
                        in0=x1[0][0:nq, :].rearrange("n (p kt) -> n kt p", kt=16), scalar1=ss[i][0:nq, 1:2],
                        scalar2=None, op0=ALU.mult),
                      reads=x1k + [("rs4", i)], writes=[("xn4", i)])
                    for half in range(2):
                        pi, pk = rpt.next()
                        for k8 in range(8):
                            kt = half * 8 + k8
                            A("pe", lambda e, pi=pi, k8=k8, kt=kt, i=i, nq=nq: e.transpose(
                                pth[pi][:, k8, 0:nq], xn[i][0:nq, kt * 128:(kt + 1) * 128], ident_b[0:nq, 0:nq]),
                              reads=[("xn4", i), "ident_b"], writes=[pk])
                        ti, tk = rtm.next()
                        for (lo, hi, r) in segs:
                            w = hi - lo
                            A("dve", lambda e, pi=pi, ti=ti, lo=lo, hi=hi, r=r, w=w, half=half: e.tensor_tensor(
                                out=tmpf[ti][:, :, lo:hi], in0=pth[pi][:, :, lo:hi],
                                in1=AfT[:, r, half * 8:half * 8 + 8].unsqueeze(2).to_broadcast([128, 8, w]), op=ALU.mult),
                              reads=[pk, "AfT"], writes=[tk])
                            A("pool", lambda e, ti=ti, lo=lo, hi=hi, r=r, w=w, half=half, off=off: e.tensor_tensor(
                                out=h2T[:, half * 8:half * 8 + 8, off + lo:off + hi], in0=tmpf[ti][:, :, lo:hi],
                                in1=BfT[:, r, half * 8:half * 8 + 8].unsqueeze(2).to_broadcast([128, 8, w]), op=ALU.add),
                              reads=[tk, "BfT"], writes=[("h2T", tl)])
                    for kt in range(16):
                        A("pe", lambda e, kt=kt, off=off, nq=nq: e.matmul(pr[0:nq, :], lhsT=h2T[:, kt, off:off + nq], rhs=wr[:, kt, :],
                                                                      start=(kt == 0), stop=(kt == 15)),
                          reads=[("h2T", tl), "wr"], writes=["pr"])
                    R_ = rt[i]
                    Q_ = rs_[i]
                    rk = ("rt", i)
                    v3 = lambda ap: ap.rearrange("p (g k) -> p g k", g=8)
                    A("act", lambda e, R_=R_, nq=nq: e.activation(out=R_[0:nq, 0, :], in_=pr[0:nq, :], func=AF.Sigmoid),
                      reads=["pr"], writes=[rk])
                    A("dve", lambda e, R_=R_, nq=nq: e.tensor_tensor(out=R_[0:nq, 1, :], in0=R_[0:nq, 0, :], in1=rbias[0:nq, :],
                                                                     op=ALU.add), reads=[rk, "rbias"], writes=[rk])
                    A("dve", lambda e, R_=R_, Q_=Q_, nq=nq: e.tensor_reduce(out=Q_[0:nq, 0:8], in_=v3(R_[0:nq, 1, :]), axis=AX.X,
                                                                            op=ALU.max), reads=[rk], writes=[rk])
                    A("dve", lambda e, R_=R_, Q_=Q_, nq=nq: e.tensor_tensor(
                        out=v3(R_[0:nq, 2, :]), in0=v3(R_[0:nq, 1, :]), in1=Q_[0:nq, 0:8].unsqueeze(2).to_broadcast([nq, 8, 8]),
                        op=ALU.is_equal), reads=[rk], writes=[rk])
                    A("dve", lambda e, R_=R_, nq=nq: e.scalar_tensor_tensor(
                        out=R_[0:nq, 2, :], in0=R_[0:nq, 2, :], scalar=-BIG, in1=R_[0:nq, 1, :], op0=ALU.mult, op1=ALU.add),
                      reads=[rk], writes=[rk])
                    A("dve", lambda e, R_=R_, Q_=Q_, nq=nq: e.tensor_reduce(out=Q_[0:nq, 8:16], in_=v3(R_[0:nq, 2, :]), axis=AX.X,
                                                                            op=ALU.max), reads=[rk], writes=[rk])
                    A("dve", lambda e, Q_=Q_, nq=nq: e.tensor_tensor(out=Q_[0:nq, 16:24], in0=Q_[0:nq, 0:8], in1=Q_[0:nq, 8:16],
                                                                     op=ALU.add), reads=[rk], writes=[rk])
                    A("dve", lambda e, Q_=Q_, nq=nq: e.max(out=Q_[0:nq, 24:32], in_=Q_[0:nq, 16:24]), reads=[rk], writes=[rk])
                    A("dve", lambda e, Q_=Q_, nq=nq: e.tensor_scalar(out=Q_[0:nq, 32:40], in0=Q_[0:nq, 16:24], scalar1=Q_[0:nq, 27:28],
                                                                     scalar2=None, op0=ALU.is_ge), reads=[rk], writes=[rk])
                    A("dve", lambda e, Q_=Q_, nq=nq: e.tensor_scalar(out=Q_[0:nq, 40:48], in0=Q_[0:nq, 32:40], scalar1=BIG, scalar2=-BIG,
                                                                     op0=ALU.mult, op1=ALU.add), reads=[rk], writes=[rk])
                    A("dve", lambda e, R_=R_, Q_=Q_, nq=nq: e.tensor_tensor(
                        out=v3(R_[0:nq, 2, :]), in0=v3(R_[0:nq, 1, :]), in1=Q_[0:nq, 32:40].unsqueeze(2).to_broadcast([nq, 8, 8]),
                        op=ALU.mult), reads=[rk], writes=[rk])
                    A("dve", lambda e, R_=R_, Q_=Q_, nq=nq: e.tensor_tensor(
                        out=v3(R_[0:nq, 2, :]), in0=v3(R_[0:nq, 2, :]), in1=Q_[0:nq, 40:48].unsqueeze(2).to_broadcast([nq, 8, 8]),
                        op=ALU.add), reads=[rk], writes=[rk])
                    A("dve", lambda e, R_=R_, Q_=Q_, nq=nq: e.max(out=Q_[0:nq, 0:8], in_=R_[0:nq, 2, :]), reads=[rk], writes=[rk])
                    A("dve", lambda e, R_=R_, Q_=Q_, nq=nq: e.tensor_scalar(out=R_[0:nq, 3, :], in0=R_[0:nq, 2, :],
                                                                            scalar1=Q_[0:nq, 7:8], scalar2=None, op0=ALU.is_ge),
                      reads=[rk], writes=[rk])
                    A("dve", lambda e, R_=R_, nq=nq: e.tensor_tensor(out=R_[0:nq, 3, :], in0=R_[0:nq, 3, :], in1=R_[0:nq, 0, :],
                                                                     op=ALU.mult), reads=[rk], writes=[rk])
                    A("dve", lambda e, R_=R_, Q_=Q_, nq=nq: e.reduce_sum(out=Q_[0:nq, 8:9], in_=R_[0:nq, 3, :], axis=AX.X),
                      reads=[rk], writes=[rk])
                    A("dve", lambda e, Q_=Q_, nq=nq: e.reciprocal(out=Q_[0:nq, 9:10], in_=Q_[0:nq, 8:9]), reads=[rk], writes=[rk])
                    A("dve", lambda e, R_=R_, Q_=Q_, nq=nq: e.tensor_scalar(out=R_[0:nq, 4, :], in0=R_[0:nq, 3, :],
                                                                            scalar1=Q_[0:nq, 9:10], scalar2=2.5, op0=ALU.mult,
                                                                            op1=ALU.mult), reads=[rk], writes=[rk])
                    A("pe", lambda e, R_=R_, nq=nq: e.transpose(pg[0:NEXP, 0:nq], R_[0:nq, 4, :], ident_f[0:nq, 0:nq]),
                      reads=[rk, "ident_f"], writes=["pg"])
                    A("act", lambda e, nq=nq, off=off: e.activation(out=gatesT[0:NEXP, off:off + nq], in_=pg[0:NEXP, 0:nq], func=AF.Copy),
                      reads=["pg"], writes=["gatesT"])
                S.emit()
            with contextlib.ExitStack() as yes:
                Yacc = yes.enter_context(nc.sbuf_tensor("Yacc_h%d" % hf, [128, 9, D], F32))
                with contextlib.ExitStack() as es:
                    def sb(name, shape, dt):
                        return es.enter_context(nc.sbuf_tensor(name + "_h%d" % hf, list(shape), dt))

                    def ps(name, shape, dt=F32):
                        return es.enter_context(nc.psum_tensor(name + "_h%d" % hf, list(shape), dt))

                    wsl = [sb("wsl%d" % i, [128, 8192], BF16) for i in range(3)]
                    stg = [sb("stg%d" % i, [128, 2048], F32) for i in range(3)]
                    rstg = Rot("stg", 3)
                    ncast = [0]
                    GU = [sb("GU%d" % i, [128, 4, 512], BF16) for i in range(2)]
                    sgt = [sb("sgt%d" % i, [128, 512], F32) for i in range(2)]
                    t1 = [sb("t1_%d" % i, [128, 512], F32) for i in range(2)]
                    gbs = [sb("gbs%d" % i, [128, 512], F32) for i in range(2)]
                    pG = [ps("pG%d" % i, [128, 512]) for i in range(2)]
                    pU = [ps("pU%d" % i, [128, 512]) for i in range(2)]
                    pgb = ps("pgb", [128, 512])
                    pY = [ps("pY%d" % i, [128, 512]) for i in range(3)]
                    rws, rGU, rsg, rt1, rgb, rpG, rpY = (Rot("wsl", 3), Rot("GU", 2), Rot("sgt", 2), Rot("t1", 2), Rot("gbs", 2),
                                                         Rot("pGU", 2), Rot("pY", 3))
                    hkeys = [("h2T", t[0]) for t in tiles]
                    for ex in range(NEXP + 1):
                        if ex < NEXP:
                            srcs = (w_gate[ex], w_up[ex], w_down[ex])
                        else:
                            srcs = (ws_gate, ws_up, ws_down)
                        wi = []
                        for k_, src in enumerate(srcs):
                            si, sk = rws.next()
                            if k_ < 2:
                                view = wsl[si][:].rearrange("p (kt f) -> p kt f", kt=16)
                                srcv = src.rearrange("(p kt) f -> p kt f", kt=16)
                            else:
                                view = wsl[si][:].rearrange("p (ft d) -> p ft d", ft=4)
                                srcv = src.rearrange("(ft p) d -> p ft d", p=128)
                            keys = []
                            for c in range(4):
                                gi_, gk_ = rstg.next()
                                if k_ < 2:
                                    sv = stg[gi_][:].rearrange("p (a f) -> p a f", a=4)
                                    sin, dv = srcv[:, 4 * c:4 * c + 4, :], view[:, 4 * c:4 * c + 4, :]
                                else:
                                    sv = stg[gi_][:]
                                    sin, dv = srcv[:, c, :], view[:, c, :]
                                A("sp", lambda e, sv=sv, sin=sin: e.dma_start(out=sv, in_=sin), writes=[gk_], dma=True)
                                ncast[0] += 1
                                ck = (sk, c)
                                if ncast[0] % 2 == 0:
                                    A("act", lambda e, sv=sv, dv=dv: e.activation(out=dv, in_=sv, func=AF.Copy),
                                      reads=[gk_], writes=[ck])
                                else:
                                    A("pool", lambda e, sv=sv, dv=dv: e.tensor_copy(dv, sv), reads=[gk_], writes=[ck])
                                keys.append(ck)
                            wi.append((view, keys))
                        (wg, wgk), (wu, wuk), (wd, wdk) = wi
                        for (b0, nb) in blocks:
                            A("pe", lambda e, ex=ex, b0=b0, nb=nb: e.matmul(
                                pgb[:, 0:nb], lhsT=ident_f[0:65, ex:ex + 1].to_broadcast([65, 128]), rhs=gatesT[0:65, b0:b0 + nb],
                                start=True, stop=True), reads=["gatesT", "ident_f"], writes=["pgb"])
                            gbi, gbk = rgb.next()
                            A("act", lambda e, gbi=gbi, nb=nb: e.activation(out=gbs[gbi][:, 0:nb], in_=pgb[:, 0:nb], func=AF.Copy),
                              reads=["pgb"], writes=[gbk])
                            gi, gk = rGU.next()
                            for ft in range(4):
                                pi, pk = rpG.next()
                                for kt in range(16):
                                    A("pe", lambda e, pi=pi, kt=kt, ft=ft, b0=b0, nb=nb, wg=wg: e.matmul(
                                        pG[pi][:, 0:nb], lhsT=wg[:, kt, ft * 128:(ft + 1) * 128], rhs=h2T[:, kt, b0:b0 + nb],
                                        start=(kt == 0), stop=(kt == 15)), reads=[wgk[kt // 4]] + hkeys, writes=[("pG", pi)])
                                for kt in range(16):
                                    A("pe", lambda e, pi=pi, kt=kt, ft=ft, b0=b0, nb=nb, wu=wu: e.matmul(
                                        pU[pi][:, 0:nb], lhsT=wu[:, kt, ft * 128:(ft + 1) * 128], rhs=h2T[:, kt, b0:b0 + nb],
                                        start=(kt == 0), stop=(kt == 15)), reads=[wuk[kt // 4]] + hkeys, writes=[("pU", pi)])
                                sgi, sgk = rsg.next()
                                A("act", lambda e, pi=pi, sgi=sgi, nb=nb: e.activation(out=sgt[sgi][:, 0:nb], in_=pG[pi][:, 0:nb],
                                                                                       func=AF.Silu), reads=[("pG", pi)], writes=[sgk])
                                ti, tk = rt1.next()
                                A("dve", lambda e, pi=pi, sgi=sgi, ti=ti, nb=nb: e.tensor_tensor(
                                    out=t1[ti][:, 0:nb], in0=pU[pi][:, 0:nb], in1=sgt[sgi][:, 0:nb], op=ALU.mult),
                                  reads=[("pU", pi), sgk], writes=[tk])
                                A("pool", lambda e, ti=ti, gi=gi, ft=ft, gbi=gbi, nb=nb: e.tensor_tensor(
                                    out=GU[gi][:, ft, 0:nb], in0=t1[ti][:, 0:nb], in1=gbs[gbi][:, 0:nb], op=ALU.mult),
                                  reads=[tk, gbk], writes=[gk])
                            for (tl, nq, off, tok0, xsrc, segs) in tiles:
                                if not (b0 <= off < b0 + nb):
                                    continue
                                lo = off - b0
                                for db in range(4):
                                    yi, yk = rpY.next()
                                    for ft in range(4):
                                        A("pe", lambda e, yi=yi, ft=ft, gi=gi, lo=lo, nq=nq, db=db, wd=wd: e.matmul(
                                            pY[yi][0:nq, :], lhsT=GU[gi][:, ft, lo:lo + nq], rhs=wd[:, ft, db * 512:(db + 1) * 512],
                                            start=(ft == 0), stop=(ft == 3)), reads=[gk, wdk[ft]], writes=[yk])
                                    if ex == 0:
                                        A("dve", lambda e, yi=yi, tl=tl, db=db, nq=nq: e.tensor_copy(
                                            Yacc[0:nq, tl, db * 512:(db + 1) * 512], pY[yi][0:nq, :]), reads=[yk], writes=[("Y", tl, db)])
                                    else:
                                        A("dve", lambda e, yi=yi, tl=tl, db=db, nq=nq: e.tensor_tensor(
                                            out=Yacc[0:nq, tl, db * 512:(db + 1) * 512], in0=pY[yi][0:nq, :],
                                            in1=Yacc[0:nq, tl, db * 512:(db + 1) * 512], op=ALU.add),
                                          reads=[yk, ("Y", tl, db)], writes=[("Y", tl, db)])
                    S.emit()
                with contextlib.ExitStack() as es:
                    def sb(name, shape, dt):
                        return es.enter_context(nc.sbuf_tensor(name + "_h%d" % hf, list(shape), dt))

                    gfb = [sb("gfb%d" % i, [128, D], F32) for i in range(2)]
                    fgb = sb("fgb", [128, D], F32)
                    x1t = [sb("x1t%d" % i, [128, D], F32) for i in range(2)]
                    yt = [sb("yt%d" % i, [128, D], F32) for i in range(2)]
                    junk = sb("junk5", [128, D], BF16)
                    ss = [sb("ss5%d" % i, [128, 2], F32) for i in range(2)]
                    A("sp", lambda e: e.dma_start(out=gfb[0][:], in_=modS[0:1, 10240:12288].partition_broadcast(128)),
                      writes=[("gfb", 0)], dma=True)
                    if hf == 1:
                        for r in range(2):
                            A("sp", lambda e, r=r: e.dma_start(out=gfb[1][16 * r:16 * r + 16, :],
                                                               in_=modS[1 + r:2 + r, 10240:12288].partition_broadcast(16)),
                              writes=[("gfb", 1)], dma=True)
                    A("sp", lambda e: e.dma_start(out=fgb[:], in_=final_g.partition_broadcast(128)), writes=["fgb"], dma=True)
                    for (tl, nq, off, tok0, xsrc, segs) in tiles:
                        i = tl % 2
                        gi = 1 if nq == 32 else 0
                        A("sp", lambda e, i=i, nq=nq, tok0=tok0: e.dma_start(out=x1t[i][0:nq, :], in_=x1S[tok0:tok0 + nq, :]),
                          writes=[("x1t", i)], dma=True)
                        A("dve", lambda e, i=i, nq=nq, tl=tl, gi=gi: e.tensor_tensor(
                            out=yt[i][0:nq, :], in0=Yacc[0:nq, tl, :], in1=gfb[gi][0:nq, :], op=ALU.mult),
                          reads=[("gfb", gi)], writes=[("yt", i)])
                        A("pool", lambda e, i=i, nq=nq: e.tensor_tensor(out=yt[i][0:nq, :], in0=yt[i][0:nq, :], in1=x1t[i][0:nq, :],
                                                                        op=ALU.add), reads=[("yt", i), ("x1t", i)], writes=[("yt", i)])
                        A("act", lambda e, i=i, nq=nq: e.activation(out=junk[0:nq, :], in_=yt[i][0:nq, :], func=AF.Square,
                                                                    accum_out=ss[i][0:nq, 0:1]), reads=[("yt", i)], writes=["junk5", ("ss5", i)])
                        A("dve", lambda e, i=i, nq=nq: e.tensor_scalar(out=ss[i][0:nq, 1:2], in0=ss[i][0:nq, 0:1], scalar1=1.0 / D,
                                                                       scalar2=EPS, op0=ALU.mult, op1=ALU.add),
                          reads=[("ss5", i)], writes=[("rs5", i)])
                        A("act", lambda e, i=i, nq=nq: e.activation(out=ss[i][0:nq, 1:2], in_=ss[i][0:nq, 1:2], func=AF.Sqrt),
                          reads=[("rs5", i)], writes=[("rs5", i)])
                        A("dve", lambda e, i=i, nq=nq: e.reciprocal(out=ss[i][0:nq, 1:2], in_=ss[i][0:nq, 1:2]),
                          reads=[("rs5", i)], writes=[("rs5", i)])
                        A("dve", lambda e, i=i, nq=nq: e.scalar_tensor_tensor(
                            out=yt[i][0:nq, :], in0=yt[i][0:nq, :], scalar=ss[i][0:nq, 1:2], in1=fgb[0:nq, :], op0=ALU.mult,
                            op1=ALU.mult), reads=[("yt", i), ("rs5", i), "fgb"], writes=[("yt", i)])
                        if nq == 32:
                            A("sp", lambda e, i=i: e.dma_start(out=y_smp, in_=yt[i][0:32, :]), reads=[("yt", i)], dma=True)
                        else:
                            A("sp", lambda e, i=i, tok0=tok0: e.dma_start(out=y_own[tok0:tok0 + 128, :], in_=yt[i][:, :]),
                              reads=[("yt", i)], dma=True)
                    S.emit()


def _bucket_np(rel):
    import jax
    import jax.numpy as jnp
    import math
    with jax.default_device(jax.devices("cpu")[0]):
        rel = jnp.asarray(rel, dtype=jnp.int32)
        nb, max_exact = 16, 8
        n = jnp.abs(rel)
        nf = jnp.maximum(n, 1).astype(jnp.float32)
        large = max_exact + (jnp.log(nf / max_exact) / math.log(128 / max_exact) * (nb - max_exact)).astype(jnp.int32)
        large = jnp.minimum(large, nb - 1)
        out = jnp.where(rel > 0, nb, 0) + jnp.where(n < max_exact, n, large)
        return np.asarray(out)


def _consts(j):
    s = np.arange(128)[:, None]
    q = np.arange(128)[None, :]
    bd = _bucket_np(s - q)
    bp = _bucket_np(s - 128 - q)
    visd = ((s // 64) <= (q // 64))
    cb = np.zeros((7, 32, 128, 128), np.float32)
    neg = np.zeros((7, 128, 128), np.float32)

    def fill(u, bk, vis):
        for b in range(32):
            m = (bk == b).astype(np.float32) - (1.0 if b == 15 else 0.0)
            cb[u, b] = m.T
        if vis is not None:
            neg[u] = np.where(vis, 0.0, NEGV)
    for v in range(5):
        delta = v - 1 - j
        if delta == 0:
            fill(v, bd, visd)
        elif delta == -1:
            fill(v, bp, None)
        elif delta > 0:
            neg[v] = NEGV
    fill(5, bp, None)
    fill(6, bd, visd)
    v01 = np.zeros((128, 4, 128), np.float32)
    vng = np.zeros((128, 4, 128), np.float32)
    for u in range(4):
        d = u - j
        if d < 0:
            v01[:, u, :] = 1.0
        elif d == 0:
            v01[:, u, :] = visd.T.astype(np.float32)
            vng[:, u, :] = np.where(visd.T, 0.0, -1e30)
        else:
            vng[:, u, :] = -1e30
    return cb.reshape(7, 32, 128 * 128), neg, v01.reshape(128, 512), vng.reshape(128, 512)


_CACHE = {}


def kernel(**inp):
    f = lambda a: np.ascontiguousarray(np.asarray(a, dtype=np.float32))
    if "nc" not in _CACHE:
        _CACHE["nc"] = build_program()
    nc = _CACHE["nc"]
    xp = f(inp["x_prompt"])
    xs = f(inp["x_sample"])
    shared = dict(
        relb=f(inp["rel_bias"]).reshape(1, 384), w_ada=f(inp["w_ada"])[0], b_ada=f(inp["b_ada"]).reshape(1, 6 * D),
        norm_a_g=f(inp["norm_a_g"]).reshape(1, D), w_in=f(inp["w_in"])[0], w_out=f(inp["w_out"])[0],
        diff_lam=f(inp["diff_lam"]).reshape(1, 512), subln_g=f(inp["subln_g"]).reshape(1, 256),
        norm_f_g=f(inp["norm_f_g"]).reshape(1, D), w_router=f(inp["w_router"])[0],
        router_bias=f(inp["router_bias"]).reshape(1, NEXP), w_gate=f(inp["w_gate"])[0], w_up=f(inp["w_up"])[0],
        w_down=f(inp["w_down"])[0], ws_gate=f(inp["ws_gate"])[0], ws_up=f(inp["ws_up"])[0], ws_down=f(inp["ws_down"])[0],
        final_g=f(inp["final_g"]).reshape(1, D))
    cp, cs = f(inp["c_prompt"]), f(inp["c_sample"])
    cak, cav, cki = f(inp["cache_a_k"])[0], f(inp["cache_a_v"])[0], f(inp["cache_a_kidx"])[0]
    cbk, cbv = f(inp["cache_b_k"])[0], f(inp["cache_b_v"])[0]
    in_maps = []
    for c in range(8):
        b, j = c // 4, c % 4
        cbc, neg, v01, vng = _consts(j)
        m = dict(shared)
        m.update(
            xb=xp[b], xown=np.ascontiguousarray(xp[b].reshape(64, 128, D)[j::4].reshape(2048, D)),
            xsmp=np.ascontiguousarray(xs[2 * c:2 * c + 2].reshape(32, D)),
            c3=np.ascontiguousarray(np.stack([cp[b], cs[2 * c], cs[2 * c + 1]])),
            cak=np.ascontiguousarray(cak[2 * c:2 * c + 2].reshape(2, PAST, 256)),
            cav=np.ascontiguousarray(cav[2 * c:2 * c + 2].reshape(2, PAST, 256)),
            cki=np.ascontiguousarray(cki[2 * c:2 * c + 2].reshape(2, PAST, 64)),
            cbk=np.ascontiguousarray(cbk[2 * c:2 * c + 2].reshape(2, PAST, 1024)),
            cbv=np.ascontiguousarray(cbv[2 * c:2 * c + 2].reshape(2, PAST, 1024)),
            cbc=cbc, negc=neg, vis01=v01, visneg=vng)
        in_maps.append(m)
    if os.environ.get("MK_CORES"):
        cs_ = [int(v) for v in os.environ["MK_CORES"].split(",")]
        res = run_bass_kernel_spmd(nc, [in_maps[c] for c in cs_], core_ids=list(range(len(cs_))),
                                   trace=bool(os.environ.get("MK_TRACE")))
        _CACHE["R"] = {c: r for c, r in zip(cs_, res.results)}
        _CACHE["res"] = res
        return None
    res = run_bass_kernel_spmd(nc, in_maps, core_ids=list(range(8)))
    R = res.results
    y_p = np.zeros((2, SEQ, D), np.float32)
    y_s = np.zeros((16, 16, D), np.float32)
    rows_p = {k: np.zeros((2, SEQ, w), np.float32) for k, w in (("ak", 256), ("av", 256), ("ki", 64), ("bk", 1024), ("bv", 1024))}
    rows_s = {k: np.zeros((16, 16, w), np.float32) for k, w in (("ak", 256), ("av", 256), ("ki", 64), ("bk", 1024), ("bv", 1024))}
    for c in range(8):
        b, j = c // 4, c % 4
        r = R[c]
        y_p[b].reshape(64, 128, D)[j::4] = np.asarray(r["y_own"]).reshape(16, 128, D)
        y_s[2 * c:2 * c + 2] = np.asarray(r["y_smp"]).reshape(2, 16, D)
        for k, w in (("ak", 256), ("av", 256), ("ki", 64), ("bk", 1024), ("bv", 1024)):
            rows_p[k][b].reshape(64, 128, w)[j::4] = np.asarray(r[k + "_own"]).reshape(16, 128, w)
            rows_s[k][2 * c:2 * c + 2] = np.asarray(r[k + "_s"]).reshape(2, 16, w)
    return (y_p, y_s,
            rows_p["ak"].reshape(1, 2, SEQ, 2, 128), rows_p["av"].reshape(1, 2, SEQ, 2, 128), rows_p["ki"].reshape(1, 2, SEQ, 64),
            rows_p["bk"].reshape(1, 2, SEQ, 4, 2, 128), rows_p["bv"].reshape(1, 2, SEQ, 4, 256),
            rows_s["ak"].reshape(1, 16, 16, 2, 128), rows_s["av"].reshape(1, 16, 16, 2, 128), rows_s["ki"].reshape(1, 16, 16, 64),
            rows_s["bk"].reshape(1, 16, 16, 4, 2, 128), rows_s["bv"].reshape(1, 16, 16, 4, 256))
```
